# Optimizing a Trainium2 kernel written in Bass

```python
import jax
import jax.numpy as jnp
from jax import lax
import numpy as np

D_MODEL = 4096
BATCH = 1
SEQ = 16384
DEPTH = 4

GRID_W = 64
CTX_LEN = 256
EPS = 1e-6
GLA_HEADS = 8
GLA_DK = 64
GLA_DV = 128
GLA_CHUNK = 64
DECAY_RANK = 16
GLA_TAU = 16.0
SG_GROUPS = 8
SG_GROUP_CH = 128
SG_CHUNK = 128
N_EXPERTS = 16
EXPERT_FF = 256
EC_CAPACITY = 2
ADA_INIT = 0.5

QK_DIM = GLA_HEADS * GLA_DK
V_DIM = GLA_HEADS * GLA_DV
SG_DIM = SG_GROUPS * SG_GROUP_CH
OFF_K = QK_DIM
OFF_V = 2 * QK_DIM
OFF_DF = OFF_V + V_DIM
OFF_DB = OFF_DF + DECAY_RANK
SCAN_COLS = OFF_DB + DECAY_RANK
OFF_U = SCAN_COLS + V_DIM
OFF_VS = OFF_U + SG_DIM
OFF_GA = OFF_VS + SG_DIM
OFF_GB = OFF_GA + D_MODEL
N_IN = OFF_GB + D_MODEL

kernel_name = 'hybrid_gla_sgu_ecmoe_dit_trunk'


def rms_norm(x, g):
    xf = x.astype(jnp.float32)
    y = xf * lax.rsqrt(jnp.mean(xf * xf, axis=-1, keepdims=True) + EPS)
    return (y * g.astype(jnp.float32)).astype(x.dtype)


def layer_norm(x, g, b):
    xf = x.astype(jnp.float32)
    xc = xf - jnp.mean(xf, axis=-1, keepdims=True)
    y = xc * lax.rsqrt(jnp.mean(xc * xc, axis=-1, keepdims=True) + EPS)
    return (y * g.astype(jnp.float32) + b.astype(jnp.float32)).astype(x.dtype)


def adaln(cond, w, b):
    m = jnp.einsum('bd,de->be', jax.nn.silu(cond), w) + b
    return jnp.split(m[:, None, :], 6, axis=-1)


def to_heads(t):
    b, l, _ = t.shape
    return t.reshape(b, l, GLA_HEADS, -1).transpose(0, 2, 1, 3).astype(jnp.float32)


def gla_inputs(p, w_dec_up, b_dec):
    q = to_heads(p[..., :OFF_K]) * (GLA_DK ** -0.5)
    k = to_heads(p[..., OFF_K:OFF_V])
    v = to_heads(p[..., OFF_V:OFF_DF])

    def log_decay(z, w, b):
        zf = jnp.einsum('blr,rk->blk', z.astype(jnp.float32), w.astype(jnp.float32)) + b.astype(jnp.float32)
        return to_heads(jax.nn.log_sigmoid(zf) / GLA_TAU)

    g_f = log_decay(p[..., OFF_DF:OFF_DB], w_dec_up[0], b_dec[0])
    g_b = log_decay(p[..., OFF_DB:SCAN_COLS], w_dec_up[1], b_dec[1])
    return q, k, v, g_f, g_b


def gla_chunk_scan(q, k, v, g, s0):
    b, h, l, dk = q.shape
    n = l // GLA_CHUNK

    def chunks(t):
        return t.reshape(b, h, n, GLA_CHUNK, t.shape[-1]).transpose(2, 0, 1, 3, 4)

    tri = jnp.tril(jnp.ones((GLA_CHUNK, GLA_CHUNK), dtype=bool))[None, None, :, :, None]

    def step(s, inp):
        qc, kc, vc, gc = inp
        bc = jnp.cumsum(gc, axis=-2)
        o_inter = jnp.einsum('bhik,bhkv->bhiv', qc * jnp.exp(bc), s)
        diff = bc[:, :, :, None, :] - bc[:, :, None, :, :]
        dec = jnp.exp(jnp.where(tri, diff, -jnp.inf))
        a = jnp.einsum('bhik,bhjk,bhijk->bhij', qc, kc, dec)
        o = o_inter + jnp.einsum('bhij,bhjv->bhiv', a, vc)
        b_last = bc[:, :, -1:, :]
        s_new = jnp.exp(b_last[:, :, 0, :])[..., None] * s + jnp.einsum(
            'bhjk,bhjv->bhkv', kc * jnp.exp(b_last - bc), vc)
        return s_new, o

    s_fin, o = lax.scan(step, s0, (chunks(q), chunks(k), chunks(v), chunks(g)))
    o = o.transpose(1, 2, 0, 3, 4).reshape(b, h, l, v.shape[-1])
    return o, s_fin


def gla_direction(ctx_in, lat_in, flip):
    rev = (lambda t: jnp.flip(t, axis=2)) if flip else (lambda t: t)
    qc, kc, vc, gc = (rev(t) for t in ctx_in)
    b, h, _, dk = qc.shape
    s0 = jnp.zeros((b, h, dk, vc.shape[-1]), jnp.float32)
    o_c, s_c = gla_chunk_scan(qc, kc, vc, gc, s0)
    qx, kx, vx, gx = (rev(t) for t in lat_in)
    o_x, _ = gla_chunk_scan(qx, kx, vx, gx, s_c)
    return rev(o_c), rev(o_x)


def gla_output(o, r, g):
    o = o * lax.rsqrt(jnp.mean(o * o, axis=-1, keepdims=True) + EPS)
    b, h, l, dv = o.shape
    o = o.transpose(0, 2, 1, 3).reshape(b, l, h * dv) * g.astype(jnp.float32)
    return (o * jax.nn.silu(r.astype(jnp.float32))).astype(r.dtype)


def spatial_gating(u, vs, ln_g, ln_b, w_s, b_s, n_chunks):
    u = jax.nn.gelu(u)
    vs = layer_norm(jax.nn.gelu(vs), ln_g, ln_b)
    b, l, _ = vs.shape
    vc = vs.reshape(b, n_chunks, l // n_chunks, SG_GROUPS, SG_GROUP_CH)
    mixed = jnp.einsum('gij,bnjgc->bnigc', w_s, vc) + b_s.T[None, None, :, :, None]
    return u * mixed.reshape(b, l, SG_DIM)


def merge_branches(p, o_gla, gla_norm_g, sg_ln_g, sg_ln_b, sg_w, sg_b,
                   w_branch_a, w_branch_b, w_out, n_chunks):
    a = gla_output(o_gla, p[..., SCAN_COLS:OFF_U], gla_norm_g)
    s = spatial_gating(p[..., OFF_U:OFF_VS], p[..., OFF_VS:OFF_GA], sg_ln_g, sg_ln_b, sg_w, sg_b, n_chunks)
    y_a = jnp.einsum('blk,kd->bld', a, w_branch_a)
    y_b = jnp.einsum('blk,kd->bld', s, w_branch_b)
    m = jax.nn.sigmoid(p[..., OFF_GA:OFF_GB]) * y_a + jax.nn.sigmoid(p[..., OFF_GB:]) * y_b
    return jnp.einsum('bld,de->ble', m, w_out)


def expert_choice_ffn(h, w_router, w1, w3, w2):
    b, n, d = h.shape
    cap = EC_CAPACITY * n // N_EXPERTS
    aff = jax.nn.softmax(jnp.einsum('bnd,de->bne', h.astype(jnp.float32),
                                    w_router.astype(jnp.float32)), axis=-1)
    gate, idx = lax.top_k(aff.transpose(0, 2, 1), cap)
    xs = jax.vmap(lambda hb, ib: hb[ib])(h, idx)
    hid = jax.nn.silu(jnp.einsum('becd,edf->becf', xs, w1)) * jnp.einsum('becd,edf->becf', xs, w3)
    y = jnp.einsum('becf,efd->becd', hid, w2) * gate[..., None].astype(h.dtype)
    return jax.vmap(lambda ib, yb: jnp.zeros((n, d), yb.dtype).at[ib.reshape(-1)].add(
        yb.reshape(-1, d)))(idx, y)


def setup_inputs(seed: int = 0) -> dict:
    key = jax.random.key(seed)
    ks = jax.random.split(key, 24)

    def nrm(k, shape, scale):
        return jax.random.normal(k, shape, jnp.float32) * scale

    L = DEPTH
    return {
        'x': nrm(ks[0], (BATCH, SEQ, D_MODEL), 1.0),
        'c': nrm(ks[1], (BATCH, D_MODEL), 1.0),
        'ctx': nrm(ks[2], (BATCH, CTX_LEN, D_MODEL), 1.0),
        'c_ctx': nrm(ks[3], (D_MODEL,), 1.0),
        'ada_w': nrm(ks[4], (L, D_MODEL, 6 * D_MODEL), ADA_INIT * D_MODEL ** -0.5),
        'ada_b': nrm(ks[5], (L, 6 * D_MODEL), 0.02),
        'norm1_g': 1.0 + nrm(ks[6], (L, D_MODEL), 0.1),
        'norm2_g': 1.0 + nrm(ks[7], (L, D_MODEL), 0.1),
        'w_in': nrm(ks[8], (L, D_MODEL, N_IN), D_MODEL ** -0.5),
        'w_dec_up': nrm(ks[9], (L, 2, DECAY_RANK, QK_DIM), DECAY_RANK ** -0.5),
        'b_dec': nrm(ks[10], (L, 2, QK_DIM), 0.5),
        'gla_norm_g': 1.0 + nrm(ks[11], (L, V_DIM), 0.1),
        'sg_ln_g': 1.0 + nrm(ks[12], (L, SG_DIM), 0.1),
        'sg_ln_b': nrm(ks[13], (L, SG_DIM), 0.02),
        'sg_w': nrm(ks[14], (L, SG_GROUPS, SG_CHUNK, SG_CHUNK), 0.5 * SG_CHUNK ** -0.5),
        'sg_b': 1.0 + nrm(ks[15], (L, SG_GROUPS, SG_CHUNK), 0.1),
        'w_branch_a': nrm(ks[16], (L, V_DIM, D_MODEL), V_DIM ** -0.5),
        'w_branch_b': nrm(ks[17], (L, SG_DIM, D_MODEL), SG_DIM ** -0.5),
        'w_out': nrm(ks[18], (L, D_MODEL, D_MODEL), D_MODEL ** -0.5),
        'w_router': nrm(ks[19], (L, D_MODEL, N_EXPERTS), D_MODEL ** -0.5),
        'w_exp1': nrm(ks[20], (L, N_EXPERTS, D_MODEL, EXPERT_FF), D_MODEL ** -0.5),
        'w_exp3': nrm(ks[21], (L, N_EXPERTS, D_MODEL, EXPERT_FF), D_MODEL ** -0.5),
        'w_exp2': nrm(ks[22], (L, N_EXPERTS, EXPERT_FF, D_MODEL), EXPERT_FF ** -0.5),
        'final_g': 1.0 + nrm(ks[23], (D_MODEL,), 0.1),
    }


def reference(x, c, ctx, c_ctx, ada_w, ada_b, norm1_g, norm2_g, w_in, w_dec_up, b_dec,
              gla_norm_g, sg_ln_g, sg_ln_b, sg_w, sg_b, w_branch_a, w_branch_b, w_out,
              w_router, w_exp1, w_exp3, w_exp2, final_g):
    rows = x.shape[1] // GRID_W
    lat_chunks = rows * GRID_W // SG_CHUNK
    ctx_chunks = ctx.shape[1] // SG_CHUNK
    for l in range(DEPTH):
        last = l == DEPTH - 1
        sh1, sc1, g1, sh2, sc2, g2 = adaln(c, ada_w[l], ada_b[l])
        csh1, csc1, cg1, csh2, csc2, cg2 = adaln(c_ctx[None], ada_w[l], ada_b[l])

        h_x = rms_norm(x, norm1_g[l]) * (1 + sc1) + sh1
        h_c = rms_norm(ctx, norm1_g[l]) * (1 + csc1) + csh1
        p_x = jnp.einsum('bld,dn->bln', h_x, w_in[l])
        w_in_c = w_in[l][:, :SCAN_COLS] if last else w_in[l]
        p_c = jnp.einsum('bld,dn->bln', h_c, w_in_c)

        qx, kx, vx, gfx, gbx = gla_inputs(p_x, w_dec_up[l], b_dec[l])
        qc, kc, vc, gfc, gbc = gla_inputs(p_c, w_dec_up[l], b_dec[l])
        o_cf, o_xf = gla_direction((qc, kc, vc, gfc), (qx, kx, vx, gfx), False)
        o_cb, o_xb = gla_direction((qc, kc, vc, gbc), (qx, kx, vx, gbx), True)

        x = x + g1 * merge_branches(p_x, o_xf + o_xb, gla_norm_g[l], sg_ln_g[l], sg_ln_b[l],
                                    sg_w[l], sg_b[l], w_branch_a[l], w_branch_b[l], w_out[l],
                                    lat_chunks)
        h2 = rms_norm(x, norm2_g[l]) * (1 + sc2) + sh2
        x = x + g2 * expert_choice_ffn(h2, w_router[l], w_exp1[l], w_exp3[l], w_exp2[l])

        if not last:
            ctx = ctx + cg1 * merge_branches(p_c, o_cf + o_cb, gla_norm_g[l], sg_ln_g[l], sg_ln_b[l],
                                             sg_w[l], sg_b[l], w_branch_a[l], w_branch_b[l],
                                             w_out[l], ctx_chunks)
            hc2 = rms_norm(ctx, norm2_g[l]) * (1 + csc2) + csh2
            ctx = ctx + cg2 * expert_choice_ffn(hc2, w_router[l], w_exp1[l], w_exp3[l], w_exp2[l])
    return rms_norm(x, final_g)
```

```python
import numpy as np
from contextlib import ExitStack
import concourse.bass as bass
import concourse.mybir as mybir
from concourse.bass_utils import run_bass_kernel_spmd

F32 = mybir.dt.float32
BF16 = mybir.dt.bfloat16
AF = mybir.ActivationFunctionType
ALU = mybir.AluOpType
AX = mybir.AxisListType

EPS = 1e-6
NH = 8
DK = 64
NE = 16
FF = 256
TB = 512
NBIS = 34
Q2 = 'pool'


class Tok:
    __slots__ = ('w', 'r')

    def __init__(self):
        self.w = None
        self.r = []


class Sched:
    def __init__(self, nc, stack):
        self.nc = nc
        self.stack = stack
        self.E = {'pe': nc.tensor, 'act': nc.scalar, 'dve': nc.vector, 'pool': nc.gpsimd, 'sp': nc.sync}
        self.sem = {e: stack.enter_context(nc.semaphore('s_' + e)) for e in self.E}
        self.cnt = {e: 0 for e in self.E}
        self.seen = {e: {} for e in self.E}
        self.lanes = {}
        self.multi_lanes = set()
        self.n_inst = 0

    def lane(self, name):
        if name not in self.lanes:
            self.lanes[name] = [self.stack.enter_context(self.nc.semaphore('d_' + name)), 0]
        return self.lanes[name]

    def _semof(self, key):
        if key in self.sem:
            return self.sem[key]
        return self.lanes[key][0]

    def emit(self, eng, fn, reads=(), writes=(), lane=None, multi=False, defer=False, skip_waw=False):
        deps = {}
        if multi:
            self.multi_lanes.add(lane)
        if lane is not None and not multi:
            ln = self.lane(lane)
            if ln[1] > 0:
                deps[lane] = ln[1]
        for t in reads:
            if t.w is not None:
                k, v = t.w
                if deps.get(k, 0) < v:
                    deps[k] = v
        for t in writes:
            if t.w is not None and not skip_waw:
                k, v = t.w
                if deps.get(k, 0) < v:
                    deps[k] = v
            for (k, v) in t.r:
                if deps.get(k, 0) < v:
                    deps[k] = v
        seen = self.seen[eng]
        e = self.E[eng]
        for k, v in deps.items():
            if eng == 'pe' and k == 'pe':
                continue
            if seen.get(k, 0) < v:
                seen[k] = v
                e.wait_ge(self._semof(k), v)
                self.n_inst += 1
        ins = fn(e)
        if lane is not None:
            ln = self.lane(lane)
            ln[1] += 16
            ticket = (lane, ln[1])
            ins.then_inc(ln[0], 16)
        elif defer:
            ticket = (eng, self.cnt[eng] + 1)
        else:
            self.cnt[eng] += 1
            ticket = (eng, self.cnt[eng])
            ins.then_inc(self.sem[eng], 1)
        for t in reads:
            if len(t.r) > 6:
                best = {}
                for (k, v) in t.r:
                    if best.get(k, 0) < v:
                        best[k] = v
                t.r = list(best.items())
            t.r.append(ticket)
        for t in writes:
            t.w = ticket
            t.r = []
        self.n_inst += 1
        return ticket

    def barrier(self):
        for eng, e in self.E.items():
            seen = self.seen[eng]
            for k, c in self.cnt.items():
                if k != eng and c > 0 and seen.get(k, 0) < c:
                    seen[k] = c
                    e.wait_ge(self.sem[k], c)
                    self.n_inst += 1
            for nm, (sem, c) in self.lanes.items():
                if nm in self.multi_lanes:
                    continue
                if c > 0 and seen.get(nm, 0) < c:
                    seen[nm] = c
                    e.wait_ge(sem, c)
                    self.n_inst += 1

    def wait_all(self, eng, toks):
        seen = self.seen[eng]
        e = self.E[eng]
        for t in toks:
            if t.w is not None:
                k, v = t.w
                if seen.get(k, 0) < v:
                    seen[k] = v
                    e.wait_ge(self._semof(k), v)


class Buf:
    def __init__(self, K, name, shape, dtype, psum=False):
        nc = K.nc
        K.uid = getattr(K, 'uid', 0) + 1
        tname = f"t{K.uid}_{name}"
        if psum:
            self.t = K.cur.enter_context(nc.psum_tensor(tname, shape, dtype))
        else:
            self.t = K.cur.enter_context(nc.sbuf_tensor(tname, shape, dtype))
        self.k = Tok()
        if psum:
            self.name = name
        else:
            self.name = f"L{K.lane_ctr}"
            K.lane_ctr += 1

    def __getitem__(self, idx):
        return self.t[idx]


class Ring:
    def __init__(self, K, name, n, shape, dtype, psum=False):
        self.b = [Buf(K, f"{name}{i}", shape, dtype, psum) for i in range(n)]
        self.i = 0

    def next(self):
        b = self.b[self.i % len(self.b)]
        self.i += 1
        return b


class Builder:
    def __init__(self, D, SEQ, CTX, L, debug=False):
        self.debug = debug
        self.D, self.SEQ, self.CTX, self.L = D, SEQ, CTX, L
        self.KC = D // 128
        self.T = CTX + SEQ
        self.NIN = 5152 + 2 * D
        self.OFF_GA = 5152
        self.OFF_GB = 5152 + D
        self.blocks = [(0, CTX, 0)] + [(CTX + TB * i, TB, 1) for i in range(SEQ // TB)]
        self.NT = self.T // 128
        assert SEQ % TB == 0 and CTX % 128 == 0 and CTX <= TB

    def declare(self):
        nc = self.nc
        D, SEQ, CTX, L, KC, T, NIN = self.D, self.SEQ, self.CTX, self.L, self.KC, self.T, self.NIN

        def inp(name, shape, dt=F32):
            return nc.dram_tensor(name, list(shape), dt, kind="ExternalInput").ap()

        def scr(name, shape, dt):
            return nc.dram_tensor(name, list(shape), dt, kind="Internal").ap()

        I = {}
        I['x'] = inp('x', [SEQ, D])
        I['ctx'] = inp('ctx', [CTX, D])
        I['ccT'] = inp('ccT', [128, KC, 2])
        I['ada_w'] = inp('ada_w', [L, D, 6 * D])
        I['ada_b'] = inp('ada_b', [L, 128, 6 * KC])
        I['n1g'] = inp('n1g', [L, 128, KC])
        I['n2g'] = inp('n2g', [L, 128, KC])
        I['fg'] = inp('fg', [128, KC])
        I['w_in'] = inp('w_in', [L, D, NIN])
        I['wup'] = inp('wup', [L, 2, 33, 512])
        I['glag'] = inp('glag', [L, 128, NH])
        I['lng'] = inp('lng', [L, 1024])
        I['lnb'] = inp('lnb', [L, 1024])
        I['wsT'] = inp('wsT', [L, 128, NH, 128])
        I['sgb'] = inp('sgb', [L, 1024])
        I['wa'] = inp('wa', [L, 1024, D])
        I['wb'] = inp('wb', [L, 1024, D])
        I['wo'] = inp('wo', [L, D, D])
        I['wr'] = inp('wr', [L, D, NE])
        I['w1'] = inp('w1', [L, NE, D, FF])
        I['w3'] = inp('w3', [L, NE, D, FF])
        I['w2'] = inp('w2', [L, NE * FF, D])
        I['ident'] = inp('ident', [128, 128])
        I['maskF'] = inp('maskF', [128, 128])
        I['maskB'] = inp('maskB', [128, 128])
        I['bsel'] = inp('bsel', [128, 128])
        I['sel'] = inp('sel', [NE, NE, 128])
        self.I = I
        self.out = nc.dram_tensor('out', [SEQ, D], F32, kind="ExternalOutput").ap()

        S = {}
        self.NCT_A = 41
        S['win'] = [scr(f's_win{l}', [41 + 2 * KC, 128, KC, 128], BF16) for l in range(L)]
        S['wa'] = [scr(f's_wa{l}', [KC, 128, 8, 128], BF16) for l in range(L)]
        S['wb'] = [scr(f's_wb{l}', [KC, 128, 8, 128], BF16) for l in range(L)]
        S['wo'] = [scr(f's_wo{l}', [KC, 128, KC, 128], BF16) for l in range(L)]
        S['wr'] = [scr(f's_wr{l}', [128, KC, NE], BF16) for l in range(L)]
        S['w1'] = [scr(f's_w1{l}', [NE * 2, 128, KC, 128], BF16) for l in range(L)]
        S['w3'] = [scr(f's_w3{l}', [NE * 2, 128, KC, 128], BF16) for l in range(L)]
        S['w2'] = [scr(f's_w2{l}', [KC, 128, 32, 128], BF16) for l in range(L)]
        S['wsT'] = [scr(f's_wsT{l}', [128, NH, 128], BF16) for l in range(L)]
        self.KCP = KC if 128 * KC * T * 4 <= 200 * 2 ** 20 else KC // 2
        self.xt_parts = [scr(f's_XT{i}', [128, self.KCP, T], F32) for i in range(KC // self.KCP)]
        S['XT'] = self.xt_parts[0]
        S['HT'] = scr('s_HT', [128, KC, T], BF16)
        S['QK'] = scr('s_QK', [64, 16, T], F32)
        S['ZL'] = scr('s_ZL', [33, T], F32)
        S['RS'] = scr('s_RS', [128, NH, T], F32)
        S['UG'] = scr('s_UG', [128, NH, T], F32)
        S['V'] = scr('s_V', [T, 1024], BF16)
        S['VSN'] = scr('s_VSN', [T, 1024], BF16)
        S['OT'] = scr('s_OT', [128, NH, T], F32)
        S['AFFL'] = scr('s_AFFL', [NE, SEQ], F32)
        S['AFFC'] = scr('s_AFFC', [NE, CTX], F32)
        S['THR'] = scr('s_THR', [128, 2], F32)
        self.Sx = S
        nb = len(self.blocks)
        nsub = {'XT': KC, 'HT': 1, 'QK': 8, 'ZL': 2, 'RS': NH, 'UG': NH, 'V': TB // 128, 'VSN': TB // 128, 'OT': 1, 'AFF': 1}
        self.dk = {name: [[Tok() for _ in range(ns)] for _ in range(nb)] for name, ns in nsub.items()}
        self.dk['THR'] = [[Tok()]]
        self.wk = {}
        for l in range(L):
            for name in ['win', 'winG', 'wa', 'wb', 'wo', 'wr', 'w1', 'w3', 'w2', 'wsT']:
                self.wk[(name, l)] = Tok()

    def coltiles_A(self):
        tiles = []
        for i in range(4):
            tiles.append(('q', i, 128 * i, 128))
        for i in range(4):
            tiles.append(('k', i, 512 + 128 * i, 128))
        for i in range(8):
            tiles.append(('v', i, 1024 + 128 * i, 128))
        tiles.append(('z', 0, 2048, 32))
        for i in range(8):
            tiles.append(('r', i, 2080 + 128 * i, 128))
        for i in range(8):
            tiles.append(('u', i, 3104 + 128 * i, 128))
        for i in range(8):
            tiles.append(('s', i, 4128 + 128 * i, 128))
        return tiles

    def emit_casts(self, l):
        K, I, S = self.K, self.I, self.Sx
        KC = self.KC

        def cast(dst, src, tok, lane):
            if len(dst.shape) == 4:
                for g in range(dst.shape[0]):
                    K.emit('pool', lambda e, g=g: e.dma_start(out=dst[g], in_=src[g]), writes=[tok], lane=lane, multi=True, skip_waw=True)
            else:
                K.emit('pool', lambda e: e.dma_start(out=dst, in_=src), writes=[tok], lane=lane, multi=True, skip_waw=True)

        win = I['w_in'][l]
        tk = self.wk[('win', l)]
        ln = f'cw{l}'
        for g0 in range(0, 16, 4):
            cast(S['win'][l][g0:g0 + 4],
                 win[:, 128 * g0:128 * (g0 + 4)].rearrange("(kc p) (g c) -> g p kc c", p=128, c=128), tk, ln)
        cast(S['win'][l][16, :, :, 0:32], win[:, 2048:2080].rearrange("(kc p) c -> p kc c", p=128), tk, ln)
        for g0 in range(0, 24, 4):
            c0 = 2080 + 128 * g0
            cast(S['win'][l][17 + g0:17 + g0 + 4],
                 win[:, c0:c0 + 512].rearrange("(kc p) (g c) -> g p kc c", p=128, c=128), tk, ln)
        tk = self.wk[('winG', l)]
        ln = f'cg{l}'
        for g0 in range(0, 2 * KC, 4):
            c0 = 5152 + 128 * g0
            cast(S['win'][l][41 + g0:41 + g0 + 4],
                 win[:, c0:c0 + 512].rearrange("(kc p) (g c) -> g p kc c", p=128, c=128), tk, ln)
        for nm in ['wa', 'wb', 'wo']:
            tk = self.wk[(nm, l)]
            for g0 in range(0, KC, 4):
                cast(S[nm][l][g0:g0 + 4],
                     I[nm][l][:, 128 * g0:128 * (g0 + 4)].rearrange("(kc p) (g c) -> g p kc c", p=128, c=128),
                     tk, f'c{nm}{l}')
        cast(S['wr'][l], I['wr'][l].rearrange("(kc p) c -> p kc c", p=128), self.wk[('wr', l)], f'cwr{l}')
        for nm in ['w1', 'w3']:
            tk = self.wk[(nm, l)]
            for e0 in range(0, NE, 2):
                for e in range(e0, e0 + 2):
                    cast(S[nm][l][2 * e:2 * e + 2],
                         I[nm][l, e].rearrange("(kc p) (g c) -> g p kc c", p=128, c=128), tk, f'c{nm}{l}')
        tk = self.wk[('w2', l)]
        for g0 in range(0, KC, 4):
            cast(S['w2'][l][g0:g0 + 4],
                 I['w2'][l][:, 128 * g0:128 * (g0 + 4)].rearrange("(kc p) (g c) -> g p kc c", p=128, c=128),
                 tk, f'cw2{l}')
        cast(S['wsT'][l], I['wsT'][l], self.wk[('wsT', l)], f'cws{l}')

    def xt(self, k0, k1, t0, t1):
        p = k0 // self.KCP
        assert (k1 - 1) // self.KCP == p
        return self.xt_parts[p][:, k0 - p * self.KCP:k1 - p * self.KCP, t0:t1]

    def xt1(self, kc, t0, t1):
        p = kc // self.KCP
        return self.xt_parts[p][:, kc - p * self.KCP, t0:t1]

    def dump(self, name, src, toks):
        if not self.debug:
            return
        dst = self.nc.dram_tensor('dbg_' + name, list(src.shape), src.dtype, kind="ExternalOutput").ap()
        flat = []
        for t in toks:
            if isinstance(t, list):
                for u in t:
                    flat.extend(u if isinstance(u, list) else [u])
            else:
                flat.append(t)
        self.K.emit(Q2, lambda e: e.dma_start(out=dst, in_=src), reads=flat, writes=[Tok()], lane='dbg')

    def phase(self):
        class P:
            def __init__(s, B):
                s.B = B

            def __enter__(s):
                s.st = ExitStack()
                s.st.__enter__()
                s.B.K.cur = s.st
                s.B.K.lane_ctr = s.B.K.lane_base
                s.B.K.barrier()
                return s

            def __exit__(s, *a):
                s.B.K.cur = s.B.K.stack
                return s.st.__exit__(*a)
        return P(self)

    def psum_pool(self, n=8):
        return Ring(self.K, 'ps', n, [128, 512], F32, psum=True)

    def blk_of_tile(self, ti):
        t0 = ti * 128
        for bi, (b0, n, s) in enumerate(self.blocks):
            if b0 <= t0 < b0 + n:
                return bi
        raise AssertionError

    def mm_group(self, ps, pout, wt, act, nk, n, M=128, w0=0):
        K = self.K
        for kc in range(nk):
            K.emit('pe', lambda e, kc=kc: e.matmul(pout, wt[:, kc, w0:w0 + M], act[:, kc, :n],
                                                   start=(kc == 0), stop=(kc == nk - 1)),
                   reads=[wt.k, act.k], writes=[ps.k], defer=(kc < nk - 1))

    def norm_stats(self, XTsrc, bi, xr, sqr, ps, rs, scale_div):
        K = self.K
        b0, n, s = self.blocks[bi]
        KC = self.KC
        G = min(4, KC)
        first = True
        for g0 in range(0, KC, G):
            xb = xr.next()
            K.emit(Q2, lambda e, xb=xb, g0=g0: e.dma_start(out=xb[:, 0:G, :n], in_=self.xt(g0, g0 + G, b0, b0 + n)),
                   reads=self.dk['XT'][bi][g0:g0 + G], writes=[xb.k], lane=xb.name)
            for j in range(G):
                sq = sqr.next()
                K.emit('act', lambda e, xb=xb, sq=sq, j=j: e.activation(sq[:, :n], xb[:, j, :n], AF.Square),
                       reads=[xb.k], writes=[sq.k])
                last = (g0 + j == KC - 1)
                K.emit('pe', lambda e, sq=sq, first=first, last=last: e.matmul(
                    ps[:, :n], self.ones_bf[:, :], sq[:, :n], start=first, stop=last),
                    reads=[sq.k, self.ones_bf.k], writes=[ps.k])
                first = False
        K.emit('act', lambda e: e.activation(rs[:, :n], ps[:, :n], AF.Sqrt, bias=self.eps_t[:, 0:1], scale=1.0 / scale_div),
               reads=[ps.k, self.eps_t.k], writes=[rs.k])
        K.emit('dve', lambda e: e.reciprocal(rs[:, :n], rs[:, :n]), reads=[rs.k], writes=[rs.k])

    def norm_apply(self, XTsrc, bi, xr, rs, A, Bv, hT, s):
        K = self.K
        b0, n, _ = self.blocks[bi]
        KC = self.KC
        G = min(4, KC)
        for g0 in range(0, KC, G):
            xb = xr.next()
            K.emit(Q2, lambda e, xb=xb, g0=g0: e.dma_start(out=xb[:, 0:G, :n], in_=self.xt(g0, g0 + G, b0, b0 + n)),
                   reads=self.dk['XT'][bi][g0:g0 + G], writes=[xb.k], lane=xb.name)
            K.emit('dve', lambda e, xb=xb: e.tensor_tensor(
                out=xb[:, 0:G, :n], in0=xb[:, 0:G, :n],
                in1=rs[:, :n].unsqueeze(1).to_broadcast([128, G, n]), op=ALU.mult),
                reads=[xb.k, rs.k], writes=[xb.k])
            for j in range(G):
                kc = g0 + j
                if Bv is None:
                    K.emit('act', lambda e, xb=xb, j=j, kc=kc: e.activation(
                        hT[:, kc, :n], xb[:, j, :n], AF.Identity, scale=A[:, kc:kc + 1]),
                        reads=[xb.k, A.k], writes=[hT.k])
                elif kc % 2 == 0:
                    K.emit('act', lambda e, xb=xb, j=j, kc=kc: e.activation(
                        hT[:, kc, :n], xb[:, j, :n], AF.Identity,
                        bias=Bv[:, kc, s:s + 1], scale=A[:, kc, s:s + 1]),
                        reads=[xb.k, A.k, Bv.k], writes=[hT.k])
                else:
                    K.emit('dve', lambda e, xb=xb, j=j, kc=kc: e.tensor_scalar(
                        out=hT[:, kc, :n], in0=xb[:, j, :n], scalar1=A[:, kc, s:s + 1],
                        scalar2=Bv[:, kc, s:s + 1], op0=ALU.mult, op1=ALU.add),
                        reads=[xb.k, A.k, Bv.k], writes=[hT.k])

    def gelu_evac(self, src_ps, src_tok, dst, dst_tok, tmpa, tmpb, n, P=128):
        K = self.K
        K.emit('act', lambda e: e.activation(tmpa[:P, :n], src_ps, AF.Copy), reads=[src_tok], writes=[tmpa.k])
        K.emit('dve', lambda e: e.tensor_tensor(out=tmpb[:P, :n], in0=tmpa[:P, :n], in1=tmpa[:P, :n], op=ALU.mult),
               reads=[tmpa.k], writes=[tmpb.k])
        K.emit('dve', lambda e: e.tensor_scalar(out=tmpb[:P, :n], in0=tmpb[:P, :n], scalar1=0.044715, scalar2=1.0,
                                                op0=ALU.mult, op1=ALU.add), reads=[tmpb.k], writes=[tmpb.k])
        K.emit('dve', lambda e: e.tensor_tensor(out=tmpb[:P, :n], in0=tmpb[:P, :n], in1=tmpa[:P, :n], op=ALU.mult),
               reads=[tmpb.k, tmpa.k], writes=[tmpb.k])
        K.emit('act', lambda e: e.activation(tmpb[:P, :n], tmpb[:P, :n], AF.Sigmoid, scale=1.5957691216057308),
               reads=[tmpb.k], writes=[tmpb.k])
        K.emit('dve', lambda e: e.tensor_tensor(out=dst, in0=tmpa[:P, :n], in1=tmpb[:P, :n], op=ALU.mult),
               reads=[tmpa.k, tmpb.k], writes=[dst_tok])

    def build(self):
        nc = bass.Bass("TRN2", target_bir_lowering=False)
        self.nc = nc
        self.declare()
        with ExitStack() as st:
            K = Sched(nc, st)
            K.stack = st
            K.cur = st
            K.lane_ctr = 0
            self.K = K
            self.consts()
            K.lane_base = K.lane_ctr
            self.emit_casts(0)
            self.prologue()
            S, dk = self.Sx, self.dk
            self.dump('XT0', S['XT'], dk['XT'])
            upto = getattr(self, 'upto', 99)
            for l in range(self.L):
                if upto < 1:
                    break
                self.adaln(l)
                if l + 1 < self.L:
                    self.emit_casts(l + 1)
                if l == 0:
                    self.dump('MOD', self.mod[:, :, :, :], [self.mod.k])
                if upto < 2:
                    break
                self.phaseA(l)
                if upto < 3:
                    break
                if l == 0:
                    for nm in ['HT', 'QK', 'ZL', 'RS', 'UG', 'V', 'VSN']:
                        self.dump(nm, S[nm], dk[nm])
                self.phaseB(l)
                if l == 0:
                    self.dump('OT', S['OT'], dk['OT'])
                if upto < 4:
                    break
                self.phaseC(l)
                if l == 0:
                    self.dump('XT1', S['XT'], dk['XT'])
                if upto < 5:
                    break
                self.phaseD(l)
                if l == 0:
                    self.dump('H2T', S['HT'], dk['HT'])
                    self.dump('AFFL', S['AFFL'], dk['AFF'])
                    self.dump('AFFC', S['AFFC'], dk['AFF'])
                if upto < 6:
                    break
                self.phaseE(l)
                if l == 0:
                    self.dump('THR', S['THR'], dk['THR'])
                if upto < 7:
                    break
                self.phaseF(l)
                if l == 0:
                    self.dump('XT2', S['XT'], dk['XT'])
            if upto >= 99:
                self.final()
            for nm, (sem, cnt) in K.lanes.items():
                if cnt > 0:
                    nc.sync.wait_ge(sem, cnt)
        return nc

    def consts(self):
        K, I = self.K, self.I
        KC = self.KC
        self.ident = Buf(K, 'ident', [128, 128], F32)
        self.maskF = Buf(K, 'maskF', [128, 128], F32)
        self.maskB = Buf(K, 'maskB', [128, 128], F32)
        self.ones_bf = Buf(K, 'ones_bf', [128, 128], BF16)
        self.ones32 = Buf(K, 'ones32', [128, 128], F32)
        self.eps_t = Buf(K, 'eps_t', [128, 1], F32)
        self.mod = Buf(K, 'mod', [128, 6, KC, 2], F32)
        self.A1 = Buf(K, 'A1', [128, KC, 2], F32)
        self.A2 = Buf(K, 'A2', [128, KC, 2], F32)
        self.cc = Buf(K, 'cc', [128, KC, 2], F32)
        self.scT = Buf(K, 'scT', [128, KC, 2], BF16)
        for b, nm in [(self.ident, 'ident'), (self.maskF, 'maskF'), (self.maskB, 'maskB')]:
            K.emit(Q2, lambda e, b=b, nm=nm: e.dma_start(out=b[:, :], in_=I[nm]), writes=[b.k], lane=b.name)
        K.emit('dve', lambda e: e.memset(self.ones_bf[:, :], 1.0), writes=[self.ones_bf.k])
        K.emit('dve', lambda e: e.memset(self.ones32[:, :], 1.0), writes=[self.ones32.k])
        K.emit('dve', lambda e: e.memset(self.eps_t[:, :], EPS), writes=[self.eps_t.k])
        K.emit(Q2, lambda e: e.dma_start(out=self.cc[:, :, :], in_=I['ccT']), writes=[self.cc.k], lane=self.cc.name)
        K.emit('act', lambda e: e.activation(self.scT[:, :, :], self.cc[:, :, :], AF.Silu),
               reads=[self.cc.k], writes=[self.scT.k])
        self.onesrow = Buf(K, 'onesrow', [1, TB], F32)
        K.emit('dve', lambda e: e.memset(self.onesrow[:, :], 1.0), writes=[self.onesrow.k])
        T = self.T
        for bi, (b0, n, s) in enumerate(self.blocks):
            K.emit(Q2, lambda e, b0=b0, n=n: e.dma_start(out=self.Sx['ZL'][32:33, b0:b0 + n], in_=self.onesrow[0:1, 0:n]),
                   reads=[self.onesrow.k], writes=[self.dk['ZL'][bi][1]], lane=self.onesrow.name)

    def prologue(self):
        K, I = self.K, self.I
        D, KC, CTX = self.D, self.KC, self.CTX
        with self.phase():
            xin = Ring(K, 'pxin', 2, [128, D], F32)
            stg = Ring(K, 'pstg', 2, [128, KC, 128], F32)
            ps = self.psum_pool(4)
            for ti in range(self.NT):
                t0 = ti * 128
                src = I['ctx'][t0:t0 + 128, :] if t0 < CTX else I['x'][t0 - CTX:t0 - CTX + 128, :]
                xb = xin.next()
                K.emit(Q2, lambda e, xb=xb, src=src: e.dma_start(out=xb[:, :], in_=src), writes=[xb.k], lane=xb.name)
                sg = stg.next()
                for c0 in range(0, KC, 4):
                    p = ps.next()
                    for j in range(4):
                        kc = c0 + j
                        K.emit('pe', lambda e, p=p, xb=xb, j=j, kc=kc: e.transpose(
                            p[:, j * 128:(j + 1) * 128], xb[:, kc * 128:(kc + 1) * 128], self.ident[:, :]),
                            reads=[xb.k, self.ident.k], writes=[p.k])
                    eng = 'act' if (c0 // 4) % 2 else 'dve'
                    if eng == 'act':
                        K.emit('act', lambda e, p=p, sg=sg, c0=c0: e.activation(
                            sg[:, c0:c0 + 4, :], p[:, :].rearrange("p (j t) -> p j t", j=4), AF.Copy),
                            reads=[p.k], writes=[sg.k])
                    else:
                        K.emit('dve', lambda e, p=p, sg=sg, c0=c0: e.tensor_copy(
                            sg[:, c0:c0 + 4, :], p[:, :].rearrange("p (j t) -> p j t", j=4)),
                            reads=[p.k], writes=[sg.k])
                bi = self.blk_of_tile(ti)
                for k0 in range(0, KC, self.KCP):
                    K.emit(Q2, lambda e, sg=sg, t0=t0, k0=k0: e.dma_start(
                        out=self.xt(k0, k0 + self.KCP, t0, t0 + 128), in_=sg[:, k0:k0 + self.KCP, :]),
                        reads=[sg.k], writes=self.dk['XT'][bi][k0:k0 + self.KCP], lane=sg.name)

    def adaln(self, l):
        K, I = self.K, self.I
        D, KC = self.D, self.KC
        with self.phase():
            wr = Ring(K, 'adw', 2, [128, KC, 512], BF16)
            adb = Buf(K, 'adb', [128, 6 * KC], F32)
            g1 = Buf(K, 'g1t', [128, KC], F32)
            g2 = Buf(K, 'g2t', [128, KC], F32)
            ps = Buf(K, 'psada', [128, 512], F32, psum=True)
            K.emit(Q2, lambda e: e.dma_start(out=adb[:, :], in_=I['ada_b'][l]), writes=[adb.k], lane=adb.name)
            K.emit(Q2, lambda e: e.dma_start(out=g1[:, :], in_=I['n1g'][l]), writes=[g1.k], lane=g1.name)
            K.emit(Q2, lambda e: e.dma_start(out=g2[:, :], in_=I['n2g'][l]), writes=[g2.k], lane=g2.name)
            ncg = 6 * D // 512
            for cg in range(ncg):
                w = wr.next()
                K.emit('pool', lambda e, w=w, cg=cg: e.dma_start(
                    out=w[:, :, :], in_=I['ada_w'][l][:, cg * 512:(cg + 1) * 512].rearrange("(kc p) c -> p kc c", p=128)),
                    writes=[w.k], lane=w.name)
                for q in range(4):
                    col = cg * 4 + q
                    for kc in range(KC):
                        K.emit('pe', lambda e, w=w, q=q, kc=kc, col=col: e.matmul(
                            ps[:, 2 * col:2 * col + 2], w[:, kc, q * 128:(q + 1) * 128], self.scT[:, kc, :],
                            start=(kc == 0), stop=(kc == KC - 1)),
                            reads=[w.k, self.scT.k], writes=[ps.k])
            K.emit('dve', lambda e: e.tensor_tensor(
                out=self.mod[:, :, :, :].rearrange("p j k s -> p (j k) s"),
                in0=ps[:, 0:12 * KC].rearrange("p (c s) -> p c s", s=2),
                in1=adb[:, :].unsqueeze(2).to_broadcast([128, 6 * KC, 2]), op=ALU.add),
                reads=[ps.k, adb.k], writes=[self.mod.k])
            for (A, g, j) in [(self.A1, g1, 1), (self.A2, g2, 4)]:
                K.emit('dve', lambda e, A=A, j=j: e.tensor_scalar(
                    out=A[:, :, :], in0=self.mod[:, j, :, :], scalar1=1.0, scalar2=None, op0=ALU.add),
                    reads=[self.mod.k], writes=[A.k])
                K.emit('dve', lambda e, A=A, g=g: e.tensor_tensor(
                    out=A[:, :, :], in0=A[:, :, :], in1=g[:, :].unsqueeze(2).to_broadcast([128, KC, 2]), op=ALU.mult),
                    reads=[A.k, g.k], writes=[A.k])

    def phaseA(self, l):
        K, I, S = self.K, self.I, self.Sx
        D, KC = self.D, self.KC
        last = (l == self.L - 1)
        tiles = self.coltiles_A()
        with self.phase():
            xr = Ring(K, 'ax', 2, [128, min(4, KC), TB], F32)
            sqr = Ring(K, 'asq', 2, [128, TB], BF16)
            rs = Buf(K, 'ars', [128, TB], F32)
            hT = Buf(K, 'ahT', [128, KC, TB], BF16)
            wring = Ring(K, 'aw', 6, [128, KC, 128], BF16)
            ps = self.psum_pool(8)
            qkst = Ring(K, 'aqk', 2, [64, 2, TB], F32)
            st32 = Ring(K, 'ast', 3, [128, TB], F32)
            tmpa = Buf(K, 'atma', [128, TB], F32)
            tmpb = Buf(K, 'atmb', [128, TB], F32)
            vT = Buf(K, 'avT', [128, NH, TB], F32)
            gS = Buf(K, 'agS', [128, NH, TB], F32)
            vtok = Ring(K, 'avtok', 2, [128, 1024], BF16)
            lnt = Ring(K, 'alnt', 2, [128, 1024], F32)
            vsn = Ring(K, 'avsn', 2, [128, 1024], BF16)
            lng = Buf(K, 'alng', [128, 1024], F32)
            lnb = Buf(K, 'alnb', [128, 1024], F32)
            stat = Buf(K, 'astat', [128, 2, 6], F32)
            mv = Buf(K, 'amv', [128, 2], F32)
            zst = Buf(K, 'azst', [32, TB], F32)
            K.emit(Q2, lambda e: e.dma_start(out=lng[:, :], in_=I['lng'][l].partition_broadcast(128)),
                   writes=[lng.k], lane=lng.name)
            K.emit(Q2, lambda e: e.dma_start(out=lnb[:, :], in_=I['lnb'][l].partition_broadcast(128)),
                   writes=[lnb.k], lane=lnb.name)
            wtok = self.wk[('win', l)]
            for bi, (b0, n, s) in enumerate(self.blocks):
                psn = ps.next()
                self.norm_stats(S['XT'], bi, xr, sqr, psn, rs, float(D))
                self.norm_apply(S['XT'], bi, xr, rs, self.A1, _ModView(self.mod, 0), hT, s)
                if not (last and s == 0):
                    K.emit(Q2, lambda e, b0=b0, n=n: e.dma_start(out=S['HT'][:, :, b0:b0 + n], in_=hT[:, :, :n]),
                           reads=[hT.k], writes=self.dk['HT'][bi], lane=hT.name)
                for ti, (kind, idx, c0, w) in enumerate(tiles):
                    if last and s == 0 and kind in ('r', 'u', 's'):
                        continue
                    wt = wring.next()
                    K.emit('sp', lambda e, wt=wt, ti=ti, w=w: e.dma_start(out=wt[:, :, 0:w], in_=S['win'][l][ti, :, :, 0:w]),
                           reads=[wtok], writes=[wt.k], lane=wt.name)
                    if kind in ('q', 'k'):
                        qs = qkst.next()
                        for half in range(2):
                            p = ps.next()
                            self.mm_group(p, p[0:64, :n], wt, hT, KC, n, M=64, w0=64 * half)
                            if half == 0:
                                K.emit('act', lambda e, p=p, qs=qs: e.activation(qs[:, 0, :n], p[0:64, :n], AF.Copy),
                                       reads=[p.k], writes=[qs.k])
                            else:
                                K.emit('dve', lambda e, p=p, qs=qs: e.tensor_copy(qs[:, 1, :n], p[0:64, :n]),
                                       reads=[p.k], writes=[qs.k])
                        hh = (0 if kind == 'q' else 8) + 2 * idx
                        K.emit(Q2, lambda e, qs=qs, hh=hh, b0=b0, n=n: e.dma_start(
                            out=S['QK'][:, hh:hh + 2, b0:b0 + n], in_=qs[:, :, :n]),
                            reads=[qs.k], writes=[self.dk['QK'][bi][hh // 2]], lane=qs.name)
                    elif kind == 'z':
                        p = ps.next()
                        self.mm_group(p, p[0:32, :n], wt, hT, KC, n, M=32, w0=0)
                        K.emit('dve', lambda e, p=p: e.tensor_copy(zst[:, :n], p[0:32, :n]), reads=[p.k], writes=[zst.k])
                        K.emit(Q2, lambda e, b0=b0, n=n: e.dma_start(out=S['ZL'][0:32, b0:b0 + n], in_=zst[:, :n]),
                               reads=[zst.k], writes=[self.dk['ZL'][bi][0]], lane=zst.name)
                    elif kind == 'v':
                        p = ps.next()
                        self.mm_group(p, p[:, :n], wt, hT, KC, n)
                        if idx % 2:
                            K.emit('act', lambda e, p=p, idx=idx: e.activation(vT[:, idx, :n], p[:, :n], AF.Copy),
                                   reads=[p.k], writes=[vT.k])
                        else:
                            K.emit('dve', lambda e, p=p, idx=idx: e.tensor_copy(vT[:, idx, :n], p[:, :n]),
                                   reads=[p.k], writes=[vT.k])
                        if idx == NH - 1:
                            for j in range(n // 128):
                                vt = vtok.next()
                                for hp in range(2):
                                    p2 = ps.next()
                                    for q in range(4):
                                        h = hp * 4 + q
                                        K.emit('pe', lambda e, p2=p2, q=q, h=h, j=j: e.transpose(
                                            p2[:, q * 128:(q + 1) * 128], vT[:, h, j * 128:(j + 1) * 128], self.ident[:, :]),
                                            reads=[vT.k, self.ident.k], writes=[p2.k])
                                    if hp:
                                        K.emit('act', lambda e, p2=p2, vt=vt: e.activation(vt[:, 512:1024], p2[:, :], AF.Copy),
                                               reads=[p2.k], writes=[vt.k])
                                    else:
                                        K.emit('dve', lambda e, p2=p2, vt=vt: e.tensor_copy(vt[:, 0:512], p2[:, :]),
                                               reads=[p2.k], writes=[vt.k])
                                K.emit(Q2, lambda e, vt=vt, j=j, b0=b0: e.dma_start(
                                    out=S['V'][b0 + j * 128:b0 + (j + 1) * 128, :], in_=vt[:, :]),
                                    reads=[vt.k], writes=[self.dk['V'][bi][j]], lane=vt.name)
                    elif kind == 'r':
                        p = ps.next()
                        self.mm_group(p, p[:, :n], wt, hT, KC, n)
                        sb = st32.next()
                        K.emit('act', lambda e, p=p, sb=sb: e.activation(sb[:, :n], p[:, :n], AF.Silu),
                               reads=[p.k], writes=[sb.k])
                        K.emit(Q2, lambda e, sb=sb, idx=idx, b0=b0, n=n: e.dma_start(
                            out=S['RS'][:, idx, b0:b0 + n], in_=sb[:, :n]),
                            reads=[sb.k], writes=[self.dk['RS'][bi][idx]], lane=sb.name)
                    elif kind == 'u':
                        p = ps.next()
                        self.mm_group(p, p[:, :n], wt, hT, KC, n)
                        sb = st32.next()
                        self.gelu_evac(p[:, :n], p.k, sb[:, :n], sb.k, tmpa, tmpb, n)
                        K.emit(Q2, lambda e, sb=sb, idx=idx, b0=b0, n=n: e.dma_start(
                            out=S['UG'][:, idx, b0:b0 + n], in_=sb[:, :n]),
                            reads=[sb.k], writes=[self.dk['UG'][bi][idx]], lane=sb.name)
                    elif kind == 's':
                        p = ps.next()
                        self.mm_group(p, p[:, :n], wt, hT, KC, n)
                        self.gelu_evac(p[:, :n], p.k, gS[:, idx, :n], gS.k, tmpa, tmpb, n)
                        if idx == NH - 1:
                            for j in range(n // 128):
                                pa = ps.next()
                                pb = ps.next()
                                for h in range(NH):
                                    pp = pa if h < 4 else pb
                                    q = h % 4
                                    K.emit('pe', lambda e, pp=pp, q=q, h=h, j=j: e.transpose(
                                        pp[:, q * 128:(q + 1) * 128], gS[:, h, j * 128:(j + 1) * 128], self.ident[:, :]),
                                        reads=[gS.k, self.ident.k], writes=[pp.k])
                                lt = lnt.next()
                                K.emit('act', lambda e, pa=pa, lt=lt: e.activation(lt[:, 0:512], pa[:, :], AF.Copy),
                                       reads=[pa.k], writes=[lt.k])
                                K.emit('act', lambda e, pb=pb, lt=lt: e.activation(lt[:, 512:1024], pb[:, :], AF.Copy),
                                       reads=[pb.k], writes=[lt.k])
                                for q in range(2):
                                    K.emit('dve', lambda e, lt=lt, q=q: e.bn_stats(stat[:, q, :], lt[:, q * 512:(q + 1) * 512]),
                                           reads=[lt.k], writes=[stat.k])
                                K.emit('dve', lambda e: e.bn_aggr(mv[:, :], stat[:, :, :].rearrange("p a b -> p (a b)")), reads=[stat.k], writes=[mv.k])
                                K.emit('act', lambda e: e.activation(mv[:, 1:2], mv[:, 1:2], AF.Sqrt, bias=self.eps_t[:, 0:1]),
                                       reads=[mv.k, self.eps_t.k], writes=[mv.k])
                                K.emit('dve', lambda e: e.reciprocal(mv[:, 1:2], mv[:, 1:2]), reads=[mv.k], writes=[mv.k])
                                K.emit('dve', lambda e, lt=lt: e.tensor_scalar(
                                    out=lt[:, :], in0=lt[:, :], scalar1=mv[:, 0:1], scalar2=mv[:, 1:2],
                                    op0=ALU.subtract, op1=ALU.mult), reads=[lt.k, mv.k], writes=[lt.k])
                                K.emit('dve', lambda e, lt=lt: e.tensor_tensor(out=lt[:, :], in0=lt[:, :], in1=lng[:, :], op=ALU.mult),
                                       reads=[lt.k, lng.k], writes=[lt.k])
                                vs = vsn.next()
                                K.emit('dve', lambda e, lt=lt, vs=vs: e.tensor_tensor(out=vs[:, :], in0=lt[:, :], in1=lnb[:, :], op=ALU.add),
                                       reads=[lt.k, lnb.k], writes=[vs.k])
                                K.emit(Q2, lambda e, vs=vs, j=j, b0=b0: e.dma_start(
                                    out=S['VSN'][b0 + j * 128:b0 + (j + 1) * 128, :], in_=vs[:, :]),
                                    reads=[vs.k], writes=[self.dk['VSN'][bi][j]], lane=vs.name)

    def phaseB(self, l):
        K, I, S = self.K, self.I, self.Sx
        NT = self.NT
        nct = self.CTX // 128
        with self.phase():
            wup = Buf(K, 'bwup', [33, 2, 512], F32)
            K.emit(Q2, lambda e: e.dma_start(out=wup[:, :, :], in_=I['wup'][l].rearrange("d r c -> r d c")),
                   writes=[wup.k], lane=wup.name)
            identb = Buf(K, 'bidb', [64, 64], F32)
            qkb = Ring(K, 'bqk', 2, [64, 16, TB], F32)
            zlb = Ring(K, 'bzl', 2, [33, TB], F32)
            vb = Ring(K, 'bv', 2, [128, TB // 128, 1024], BF16)
            ob = Ring(K, 'bo', 2, [128, NH, TB], F32)
            S32 = Buf(K, 'bS32', [64, NH, 128], F32)
            Sbf = Buf(K, 'bSbf', [64, NH, 128], BF16)
            lz = Buf(K, 'blz', [64, NH, 128], F32)
            Gl = Buf(K, 'bGl', [64, NH, 128], F32)
            bcs = Buf(K, 'bbcs', [64, NH, 128], F32)
            eq = Buf(K, 'beq', [64, NH, 128], F32)
            ek = Buf(K, 'bek', [64, NH, 128], F32)
            qe = Buf(K, 'bqe', [64, NH, 128], BF16)
            ke32 = Buf(K, 'bke32', [64, NH, 128], F32)
            kebf = Buf(K, 'bkebf', [64, NH, 128], BF16)
            ketok = Buf(K, 'bketok', [128, NH, 64], BF16)
            atm = Buf(K, 'batm', [128, NH, 128], BF16)
            pzu = [Buf(K, f'bpzu{i}', [128, 512], F32, psum=True) for i in range(2)]
            pkt = Buf(K, 'bpkt', [128, 512], F32, psum=True)
            pat = [Buf(K, f'bpat{i}', [128, 512], F32, psum=True) for i in range(2)]
            po = [Buf(K, f'bpo{i}', [128, 512], F32, psum=True) for i in range(2)]

            def v4(b):
                return b.rearrange("p (h t) -> p h t", h=4)

            for d in range(2):
                mask = self.maskF if d == 0 else self.maskB
                if d == 0:
                    order = list(range(NT))
                else:
                    order = list(range(nct - 1, -1, -1)) + list(range(NT - 1, nct - 1, -1))
                K.emit('dve', lambda e: e.memset(S32[:, :, :], 0.0), writes=[S32.k])
                K.emit('dve', lambda e: e.memset(Sbf[:, :, :], 0.0), writes=[Sbf.k])
                curb = -1
                for ti in order:
                    bi = self.blk_of_tile(ti)
                    b0, n, s = self.blocks[bi]
                    if bi != curb:
                        if curb >= 0:
                            pb0, pn, _ = self.blocks[curb]
                            K.emit(Q2, lambda e, o=o, pb0=pb0, pn=pn: e.dma_start(out=S['OT'][:, :, pb0:pb0 + pn], in_=o[:, :, :pn]),
                                   reads=[o.k], writes=self.dk['OT'][curb], lane=o.name)
                        curb = bi
                        qk = qkb.next()
                        zl = zlb.next()
                        v = vb.next()
                        o = ob.next()
                        K.emit(Q2, lambda e, qk=qk, b0=b0, n=n: e.dma_start(out=qk[:, :, :n], in_=S['QK'][:, :, b0:b0 + n]),
                               reads=self.dk['QK'][bi], writes=[qk.k], lane=qk.name)
                        K.emit(Q2, lambda e, zl=zl, b0=b0, n=n: e.dma_start(out=zl[:, :n], in_=S['ZL'][:, b0:b0 + n]),
                               reads=self.dk['ZL'][bi], writes=[zl.k], lane=zl.name)
                        K.emit(Q2, lambda e, v=v, b0=b0, n=n: e.dma_start(
                            out=v[:, 0:n // 128, :], in_=S['V'][b0:b0 + n, :].rearrange("(j p) c -> p j c", p=128)),
                            reads=self.dk['V'][bi], writes=[v.k], lane=v.name)
                        if d == 1:
                            K.emit(Q2, lambda e, o=o, b0=b0, n=n: e.dma_start(out=o[:, :, :n], in_=S['OT'][:, :, b0:b0 + n]),
                                   reads=self.dk['OT'][bi], writes=[o.k], lane=o.name)
                    j = (ti * 128 - b0) // 128
                    c0, c1 = j * 128, (j + 1) * 128
                    for h in range(NH):
                        pz = pzu[h // 4]
                        K.emit('pe', lambda e, pz=pz, h=h, zl=zl, c0=c0, c1=c1: e.matmul(
                            pz[0:64, (h % 4) * 128:(h % 4 + 1) * 128], wup[:, d, h * 64:(h + 1) * 64], zl[:, c0:c1],
                            start=True, stop=True), reads=[wup.k, zl.k], writes=[pz.k])
                    for hp in range(2):
                        K.emit('act', lambda e, hp=hp: e.activation(lz[:, hp * 4:(hp + 1) * 4, :], v4(pzu[hp][0:64, :]), AF.Exp, scale=-1.0),
                               reads=[pzu[hp].k], writes=[lz.k])
                    K.emit('act', lambda e: e.activation(lz[:, :, :], lz[:, :, :], AF.Ln, bias=self.ones32[0:64, 0:1]),
                           reads=[lz.k, self.ones32.k], writes=[lz.k])
                    for h in range(NH):
                        K.emit('dve', lambda e, h=h: e.tensor_tensor_scan(
                            out=Gl[:, h, :], data0=self.ones32[0:64, :], data1=lz[:, h, :], initial=0.0,
                            op0=ALU.mult, op1=ALU.add), reads=[lz.k, self.ones32.k], writes=[Gl.k])
                    if d == 0:
                        src = Gl
                    else:
                        K.emit('dve', lambda e: e.tensor_tensor(out=bcs[:, :, :], in0=lz[:, :, :], in1=Gl[:, :, :], op=ALU.subtract),
                               reads=[lz.k, Gl.k], writes=[bcs.k])
                        K.emit('dve', lambda e: e.tensor_tensor(
                            out=bcs[:, :, :], in0=bcs[:, :, :], in1=Gl[:, :, 127:128].to_broadcast([64, NH, 128]), op=ALU.add),
                            reads=[bcs.k, Gl.k], writes=[bcs.k])
                        src = bcs
                    K.emit('act', lambda e, src=src: e.activation(eq[:, :, :], src[:, :, :], AF.Exp, scale=-1.0 / 16.0),
                           reads=[src.k], writes=[eq.k])
                    K.emit('act', lambda e, src=src: e.activation(ek[:, :, :], src[:, :, :], AF.Exp, scale=1.0 / 16.0),
                           reads=[src.k], writes=[ek.k])
                    for hp in range(2):
                        hs = slice(hp * 4, hp * 4 + 4)
                        K.emit('dve', lambda e, hs=hs, qk=qk, c0=c0, c1=c1: e.scalar_tensor_tensor(
                            out=qe[:, hs, :], in0=qk[:, hs, c0:c1], scalar=0.125, in1=eq[:, hs, :],
                            op0=ALU.mult, op1=ALU.mult), reads=[qk.k, eq.k], writes=[qe.k])
                    K.emit('dve', lambda e, qk=qk, c0=c0, c1=c1: e.tensor_tensor(
                        out=ke32[:, :, :], in0=qk[:, 8:16, c0:c1], in1=ek[:, :, :], op=ALU.mult),
                        reads=[qk.k, ek.k], writes=[ke32.k])
                    K.emit('act', lambda e: e.activation(kebf[:, :, :], ke32[:, :, :], AF.Copy), reads=[ke32.k], writes=[kebf.k])
                    for h in range(NH):
                        K.emit('pe', lambda e, h=h: e.transpose(pkt[:, h * 64:(h + 1) * 64], ke32[:, h, :], self.ident[0:64, 0:64]),
                               reads=[ke32.k, self.ident.k], writes=[pkt.k])
                    K.emit('act', lambda e: e.activation(ketok[:, :, :], pkt[:, :].rearrange("p (h k) -> p h k", h=NH), AF.Copy),
                           reads=[pkt.k], writes=[ketok.k])
                    for h in range(NH):
                        pa = pat[h // 4]
                        K.emit('pe', lambda e, pa=pa, h=h: e.matmul(
                            pa[:, (h % 4) * 128:(h % 4 + 1) * 128], kebf[:, h, :], qe[:, h, :], start=True, stop=True),
                            reads=[kebf.k, qe.k], writes=[pa.k])
                    for hp in range(2):
                        K.emit('dve', lambda e, hp=hp, mask=mask: e.tensor_tensor(
                            out=atm[:, hp * 4:(hp + 1) * 4, :], in0=v4(pat[hp][:, :]),
                            in1=mask[:, :].unsqueeze(1).to_broadcast([128, 4, 128]), op=ALU.mult),
                            reads=[pat[hp].k, mask.k], writes=[atm.k])
                    for h in range(NH):
                        pp = po[h // 4]
                        osl = pp[:, (h % 4) * 128:(h % 4 + 1) * 128]
                        K.emit('pe', lambda e, osl=osl, h=h, v=v, j=j: e.matmul(
                            osl, v[:, j, h * 128:(h + 1) * 128], atm[:, h, :], start=True, stop=False),
                            reads=[v.k, atm.k], writes=[pp.k])
                        K.emit('pe', lambda e, osl=osl, h=h: e.matmul(
                            osl, Sbf[:, h, :], qe[:, h, :], start=False, stop=True),
                            reads=[Sbf.k, qe.k], writes=[pp.k])
                    for hp in range(2):
                        hs = slice(hp * 4, hp * 4 + 4)
                        if d == 0:
                            K.emit('act', lambda e, hp=hp, hs=hs, o=o, c0=c0, c1=c1: e.activation(
                                o[:, hs, c0:c1], v4(po[hp][:, :]), AF.Copy), reads=[po[hp].k], writes=[o.k])
                        else:
                            K.emit('dve', lambda e, hp=hp, hs=hs, o=o, c0=c0, c1=c1: e.tensor_tensor(
                                out=o[:, hs, c0:c1], in0=o[:, hs, c0:c1], in1=v4(po[hp][:, :]), op=ALU.add),
                                reads=[po[hp].k, o.k], writes=[o.k])
                    for h in range(NH):
                        pz = pzu[h // 4]
                        K.emit('pe', lambda e, pz=pz, h=h, v=v, j=j: e.matmul(
                            pz[0:64, (h % 4) * 128:(h % 4 + 1) * 128], ketok[:, h, :], v[:, j, h * 128:(h + 1) * 128],
                            start=True, stop=True), reads=[ketok.k, v.k], writes=[pz.k])
                    ecol = 127 if d == 0 else 0
                    for hp in range(2):
                        hs = slice(hp * 4, hp * 4 + 4)
                        K.emit('dve', lambda e, hp=hp, hs=hs: e.tensor_tensor(
                            out=S32[:, hs, :], in0=S32[:, hs, :], in1=v4(pzu[hp][0:64, :]), op=ALU.add),
                            reads=[pzu[hp].k, S32.k], writes=[S32.k])
                    K.emit('dve', lambda e, ecol=ecol: e.tensor_tensor(
                        out=S32[:, :, :], in0=S32[:, :, :], in1=eq[:, :, ecol:ecol + 1].to_broadcast([64, NH, 128]), op=ALU.mult),
                        reads=[S32.k, eq.k], writes=[S32.k])
                    K.emit('act', lambda e: e.activation(Sbf[:, :, :], S32[:, :, :], AF.Copy), reads=[S32.k], writes=[Sbf.k])
                pb0, pn, _ = self.blocks[curb]
                K.emit(Q2, lambda e, o=o, pb0=pb0, pn=pn: e.dma_start(out=S['OT'][:, :, pb0:pb0 + pn], in_=o[:, :, :pn]),
                       reads=[o.k], writes=self.dk['OT'][curb], lane=o.name)

    def phaseC(self, l):
        K, I, S = self.K, self.I, self.Sx
        D, KC = self.D, self.KC
        last = (l == self.L - 1)
        with self.phase():
            hT = Buf(K, 'chT', [128, KC, TB], BF16)
            mT = Buf(K, 'cmT', [128, KC, TB], BF16)
            aT = Buf(K, 'caT', [128, NH, TB], BF16)
            sT = Buf(K, 'csT', [128, NH, TB], BF16)
            w8 = Ring(K, 'cw8', 6, [128, KC, 128], BF16)
            w2 = Ring(K, 'cw2', 6, [128, 8, 128], BF16)
            ld = Ring(K, 'cld', 4, [128, TB], F32)
            t32 = Ring(K, 'ct32', 3, [128, TB], F32)
            sqb = Buf(K, 'csq', [128, TB], BF16)
            rr = Buf(K, 'crr', [128, TB], F32)
            vsn = Buf(K, 'cvsn', [128, TB // 128, 1024], BF16)
            wsT = Buf(K, 'cwsT', [128, NH, 128], BF16)
            sgb = Buf(K, 'csgb', [128, NH, 128], F32)
            glag = Buf(K, 'cglag', [128, NH], F32)
            xr = Ring(K, 'cx', 3, [128, TB], F32)
            ps = self.psum_pool(8)
            K.emit(Q2, lambda e: e.dma_start(out=wsT[:, :, :], in_=S['wsT'][l]), reads=[self.wk[('wsT', l)]],
                   writes=[wsT.k], lane=wsT.name)
            K.emit(Q2, lambda e: e.dma_start(out=sgb[:, :, :].rearrange("p g i -> p (g i)"), in_=I['sgb'][l].partition_broadcast(128)),
                   writes=[sgb.k], lane=sgb.name)
            K.emit(Q2, lambda e: e.dma_start(out=glag[:, :], in_=I['glag'][l]), writes=[glag.k], lane=glag.name)
            for bi, (b0, n, s) in enumerate(self.blocks):
                if last and s == 0:
                    continue
                K.emit(Q2, lambda e, b0=b0, n=n: e.dma_start(out=hT[:, :, :n], in_=S['HT'][:, :, b0:b0 + n]),
                       reads=self.dk['HT'][bi], writes=[hT.k], lane=hT.name)
                K.emit(Q2, lambda e, b0=b0, n=n: e.dma_start(
                    out=vsn[:, 0:n // 128, :], in_=S['VSN'][b0:b0 + n, :].rearrange("(j p) c -> p j c", p=128)),
                    reads=self.dk['VSN'][bi], writes=[vsn.k], lane=vsn.name)
                for h in range(NH):
                    oh = ld.next()
                    K.emit(Q2, lambda e, oh=oh, h=h, b0=b0, n=n: e.dma_start(out=oh[:, :n], in_=S['OT'][:, h, b0:b0 + n]),
                           reads=self.dk['OT'][bi], writes=[oh.k], lane=oh.name)
                    rh = ld.next()
                    K.emit(Q2, lambda e, rh=rh, h=h, b0=b0, n=n: e.dma_start(out=rh[:, :n], in_=S['RS'][:, h, b0:b0 + n]),
                           reads=[self.dk['RS'][bi][h]], writes=[rh.k], lane=rh.name)
                    K.emit('act', lambda e, oh=oh: e.activation(sqb[:, :n], oh[:, :n], AF.Square), reads=[oh.k], writes=[sqb.k])
                    p = ps.next()
                    K.emit('pe', lambda e, p=p: e.matmul(p[:, :n], self.ones_bf[:, :], sqb[:, :n], start=True, stop=True),
                           reads=[sqb.k, self.ones_bf.k], writes=[p.k])
                    K.emit('act', lambda e, p=p: e.activation(rr[:, :n], p[:, :n], AF.Sqrt, bias=self.eps_t[:, 0:1], scale=1.0 / 128.0),
                           reads=[p.k, self.eps_t.k], writes=[rr.k])
                    K.emit('dve', lambda e: e.reciprocal(rr[:, :n], rr[:, :n]), reads=[rr.k], writes=[rr.k])
                    K.emit('dve', lambda e, oh=oh: e.tensor_tensor(out=oh[:, :n], in0=oh[:, :n], in1=rr[:, :n], op=ALU.mult),
                           reads=[oh.k, rr.k], writes=[oh.k])
                    K.emit('dve', lambda e, oh=oh, rh=rh, h=h: e.scalar_tensor_tensor(
                        out=aT[:, h, :n], in0=oh[:, :n], scalar=glag[:, h:h + 1], in1=rh[:, :n], op0=ALU.mult, op1=ALU.mult),
                        reads=[oh.k, rh.k, glag.k], writes=[aT.k])
                for g in range(NH):
                    ug = ld.next()
                    K.emit(Q2, lambda e, ug=ug, g=g, b0=b0, n=n: e.dma_start(out=ug[:, :n], in_=S['UG'][:, g, b0:b0 + n]),
                           reads=[self.dk['UG'][bi][g]], writes=[ug.k], lane=ug.name)
                    p = ps.next()
                    for j in range(n // 128):
                        K.emit('pe', lambda e, p=p, j=j, g=g: e.matmul(
                            p[:, j * 128:(j + 1) * 128], vsn[:, j, g * 128:(g + 1) * 128], wsT[:, g, :], start=True, stop=True),
                            reads=[vsn.k, wsT.k], writes=[p.k])
                    tt = t32.next()
                    nj = n // 128
                    K.emit('dve', lambda e, p=p, tt=tt, g=g, nj=nj: e.tensor_tensor(
                        out=tt[:, :n].rearrange("p (j i) -> p j i", j=nj), in0=p[:, :n].rearrange("p (j i) -> p j i", j=nj),
                        in1=sgb[:, g, :].unsqueeze(1).to_broadcast([128, nj, 128]), op=ALU.add),
                        reads=[p.k, sgb.k], writes=[tt.k])
                    K.emit('dve', lambda e, tt=tt, ug=ug, g=g: e.tensor_tensor(out=sT[:, g, :n], in0=tt[:, :n], in1=ug[:, :n], op=ALU.mult),
                           reads=[tt.k, ug.k], writes=[sT.k])
                for ncx in range(KC):
                    wga = w8.next()
                    K.emit('sp', lambda e, wga=wga, ncx=ncx: e.dma_start(out=wga[:, :, :], in_=S['win'][l][41 + ncx]),
                           reads=[self.wk[('winG', l)]], writes=[wga.k], lane=wga.name)
                    wgb = w8.next()
                    K.emit('sp', lambda e, wgb=wgb, ncx=ncx: e.dma_start(out=wgb[:, :, :], in_=S['win'][l][41 + KC + ncx]),
                           reads=[self.wk[('winG', l)]], writes=[wgb.k], lane=wgb.name)
                    wa = w2.next()
                    K.emit('sp', lambda e, wa=wa, ncx=ncx: e.dma_start(out=wa[:, :, :], in_=S['wa'][l][ncx]),
                           reads=[self.wk[('wa', l)]], writes=[wa.k], lane=wa.name)
                    wb = w2.next()
                    K.emit('sp', lambda e, wb=wb, ncx=ncx: e.dma_start(out=wb[:, :, :], in_=S['wb'][l][ncx]),
                           reads=[self.wk[('wb', l)]], writes=[wb.k], lane=wb.name)
                    pga, pgb, pya, pyb = ps.next(), ps.next(), ps.next(), ps.next()
                    self.mm_group(pga, pga[:, :n], wga, hT, KC, n)
                    self.mm_group(pya, pya[:, :n], wa, aT, 8, n)
                    self.mm_group(pgb, pgb[:, :n], wgb, hT, KC, n)
                    self.mm_group(pyb, pyb[:, :n], wb, sT, 8, n)
                    ta = t32.next()
                    tb = t32.next()
                    K.emit('act', lambda e, pga=pga, ta=ta: e.activation(ta[:, :n], pga[:, :n], AF.Sigmoid), reads=[pga.k], writes=[ta.k])
                    K.emit('act', lambda e, pgb=pgb, tb=tb: e.activation(tb[:, :n], pgb[:, :n], AF.Sigmoid), reads=[pgb.k], writes=[tb.k])
                    K.emit('dve', lambda e, ta=ta, pya=pya: e.tensor_tensor(out=ta[:, :n], in0=ta[:, :n], in1=pya[:, :n], op=ALU.mult),
                           reads=[ta.k, pya.k], writes=[ta.k])
                    K.emit('dve', lambda e, tb=tb, pyb=pyb: e.tensor_tensor(out=tb[:, :n], in0=tb[:, :n], in1=pyb[:, :n], op=ALU.mult),
                           reads=[tb.k, pyb.k], writes=[tb.k])
                    K.emit('dve', lambda e, ta=ta, tb=tb, ncx=ncx: e.tensor_tensor(out=mT[:, ncx, :n], in0=ta[:, :n], in1=tb[:, :n], op=ALU.add),
                           reads=[ta.k, tb.k], writes=[mT.k])
                for ec in range(KC):
                    wo = w8.next()
                    K.emit('sp', lambda e, wo=wo, ec=ec: e.dma_start(out=wo[:, :, :], in_=S['wo'][l][ec]),
                           reads=[self.wk[('wo', l)]], writes=[wo.k], lane=wo.name)
                    xb = xr.next()
                    K.emit(Q2, lambda e, xb=xb, ec=ec, b0=b0, n=n: e.dma_start(out=xb[:, :n], in_=self.xt1(ec, b0, b0 + n)),
                           reads=[self.dk['XT'][bi][ec]], writes=[xb.k], lane=xb.name)
                    p = ps.next()
                    self.mm_group(p, p[:, :n], wo, mT, KC, n)
                    K.emit('dve', lambda e, p=p, xb=xb, ec=ec, s=s: e.scalar_tensor_tensor(
                        out=xb[:, :n], in0=p[:, :n], scalar=self.mod[:, 2, ec, s:s + 1], in1=xb[:, :n], op0=ALU.mult, op1=ALU.add),
                        reads=[p.k, xb.k, self.mod.k], writes=[xb.k])
                    K.emit(Q2, lambda e, xb=xb, ec=ec, b0=b0, n=n: e.dma_start(out=self.xt1(ec, b0, b0 + n), in_=xb[:, :n]),
                           reads=[xb.k], writes=[self.dk['XT'][bi][ec]], lane=xb.name)

    def phaseD(self, l):
        K, I, S = self.K, self.I, self.Sx
        D, KC = self.D, self.KC
        last = (l == self.L - 1)
        with self.phase():
            xr = Ring(K, 'dx', 2, [128, min(4, KC), TB], F32)
            sqr = Ring(K, 'dsq', 2, [128, TB], BF16)
            rs = Buf(K, 'drs', [128, TB], F32)
            hT = Ring(K, 'dhT', 2, [128, KC, TB], BF16)
            wrt = Buf(K, 'dwr', [128, KC, NE], BF16)
            ex = Ring(K, 'dex', 2, [NE, TB], F32)
            rsum = Buf(K, 'drsum', [NE, TB], F32)
            ps = self.psum_pool(6)
            K.emit(Q2, lambda e: e.dma_start(out=wrt[:, :, :], in_=S['wr'][l]), reads=[self.wk[('wr', l)]],
                   writes=[wrt.k], lane=wrt.name)
            for bi, (b0, n, s) in enumerate(self.blocks):
                if last and s == 0:
                    continue
                psn = ps.next()
                h = hT.next()
                self.norm_stats(S['XT'], bi, xr, sqr, psn, rs, float(D))
                self.norm_apply(S['XT'], bi, xr, rs, self.A2, _ModView(self.mod, 3), h, s)
                K.emit(Q2, lambda e, h=h, b0=b0, n=n: e.dma_start(out=S['HT'][:, :, b0:b0 + n], in_=h[:, :, :n]),
                       reads=[h.k], writes=self.dk['HT'][bi], lane=h.name)
                p = ps.next()
                self.mm_group(p, p[0:NE, :n], wrt, h, KC, n, M=NE, w0=0)
                x = ex.next()
                K.emit('act', lambda e, p=p, x=x: e.activation(x[:, :n], p[0:NE, :n], AF.Exp), reads=[p.k], writes=[x.k])
                p2 = ps.next()
                K.emit('pe', lambda e, p2=p2, x=x: e.matmul(p2[0:NE, :n], self.ones32[0:NE, 0:NE], x[:, :n], start=True, stop=True),
                       reads=[x.k, self.ones32.k], writes=[p2.k])
                K.emit('dve', lambda e, p2=p2: e.reciprocal(rsum[:, :n], p2[0:NE, :n]), reads=[p2.k], writes=[rsum.k])
                K.emit('dve', lambda e, x=x: e.tensor_tensor(out=x[:, :n], in0=x[:, :n], in1=rsum[:, :n], op=ALU.mult),
                       reads=[x.k, rsum.k], writes=[x.k])
                affd = S['AFFC'][:, 0:n] if s == 0 else S['AFFL'][:, b0 - self.CTX:b0 - self.CTX + n]
                K.emit(Q2, lambda e, x=x, affd=affd, n=n: e.dma_start(out=affd, in_=x[:, :n]),
                       reads=[x.k], writes=self.dk['AFF'][bi], lane=x.name)

    def phaseE(self, l):
        K, I, S = self.K, self.I, self.Sx
        SEQ, CTX = self.SEQ, self.CTX
        last = (l == self.L - 1)
        nl, ncx = SEQ // 8, CTX // 8
        with self.phase():
            aff = Buf(K, 'eaff', [128, nl + ncx], F32)
            junk = Buf(K, 'ejunk', [128, nl], F32)
            bsel = Buf(K, 'ebsel', [128, 128], F32)
            lo = Buf(K, 'elo', [128, 2], F32)
            hi = Buf(K, 'ehi', [128, 2], F32)
            mid = Buf(K, 'emid', [128, 2], F32)
            cnt = Buf(K, 'ecnt', [128, 2], F32)
            ge = Buf(K, 'ege', [128, 2], F32)
            dd = Buf(K, 'edd', [128, 2], F32)
            kv = Buf(K, 'ekv', [128, 2], F32)
            pc = Buf(K, 'epc', [128, 512], F32, psum=True)
            K.emit(Q2, lambda e: e.dma_start(out=bsel[:, :], in_=I['bsel']), writes=[bsel.k], lane=bsel.name)
            K.emit(Q2, lambda e: e.dma_start(out=aff[:, 0:nl], in_=S['AFFL'].rearrange("e (s i) -> (e s) i", s=8)),
                   reads=[t for b in self.dk['AFF'][1:] for t in b], writes=[aff.k], lane=aff.name)
            if not last:
                K.emit(Q2, lambda e: e.dma_start(out=aff[:, nl:nl + ncx], in_=S['AFFC'].rearrange("e (s i) -> (e s) i", s=8)),
                       reads=self.dk['AFF'][0], writes=[aff.k], lane=aff.name)
            else:
                K.emit('dve', lambda e: e.memset(aff[:, nl:nl + ncx], 0.0), writes=[aff.k])
            K.emit('dve', lambda e: e.memset(lo[:, :], 0.0), writes=[lo.k])
            K.emit('dve', lambda e: e.memset(hi[:, :], 2.0), writes=[hi.k])
            K.emit('dve', lambda e: e.memset(kv[:, 0:1], float(SEQ // 8)), writes=[kv.k])
            K.emit('dve', lambda e: e.memset(kv[:, 1:2], float(CTX // 8)), writes=[kv.k])
            for it in range(NBIS):
                K.emit('dve', lambda e: e.tensor_tensor(out=mid[:, :], in0=lo[:, :], in1=hi[:, :], op=ALU.add),
                       reads=[lo.k, hi.k], writes=[mid.k])
                K.emit('dve', lambda e: e.tensor_scalar(out=mid[:, :], in0=mid[:, :], scalar1=0.5, scalar2=None, op0=ALU.mult),
                       reads=[mid.k], writes=[mid.k])
                K.emit('dve', lambda e: e.tensor_scalar(out=junk[:, 0:nl], in0=aff[:, 0:nl], scalar1=mid[:, 0:1], scalar2=0.0,
                                                        op0=ALU.is_ge, op1=ALU.add, accum_out=cnt[:, 0:1]),
                       reads=[aff.k, mid.k], writes=[junk.k, cnt.k])
                K.emit('dve', lambda e: e.tensor_scalar(out=junk[:, 0:ncx], in0=aff[:, nl:nl + ncx], scalar1=mid[:, 1:2], scalar2=0.0,
                                                        op0=ALU.is_ge, op1=ALU.add, accum_out=cnt[:, 1:2]),
                       reads=[aff.k, mid.k], writes=[junk.k, cnt.k])
                K.emit('pe', lambda e: e.matmul(pc[:, 0:2], bsel[:, :], cnt[:, :], start=True, stop=True),
                       reads=[bsel.k, cnt.k], writes=[pc.k])
                K.emit('dve', lambda e: e.tensor_tensor(out=ge[:, :], in0=pc[:, 0:2], in1=kv[:, :], op=ALU.is_ge),
                       reads=[pc.k, kv.k], writes=[ge.k])
                K.emit('dve', lambda e: e.tensor_tensor(out=dd[:, :], in0=mid[:, :], in1=lo[:, :], op=ALU.subtract),
                       reads=[mid.k, lo.k], writes=[dd.k])
                K.emit('dve', lambda e: e.tensor_tensor(out=dd[:, :], in0=dd[:, :], in1=ge[:, :], op=ALU.mult),
                       reads=[dd.k, ge.k], writes=[dd.k])
                K.emit('dve', lambda e: e.tensor_tensor(out=lo[:, :], in0=lo[:, :], in1=dd[:, :], op=ALU.add),
                       reads=[lo.k, dd.k], writes=[lo.k])
                K.emit('dve', lambda e: e.tensor_tensor(out=dd[:, :], in0=hi[:, :], in1=mid[:, :], op=ALU.subtract),
                       reads=[mid.k, hi.k], writes=[dd.k])
                K.emit('dve', lambda e: e.tensor_tensor(out=dd[:, :], in0=dd[:, :], in1=ge[:, :], op=ALU.mult),
                       reads=[dd.k, ge.k], writes=[dd.k])
                K.emit('dve', lambda e: e.tensor_tensor(out=hi[:, :], in0=mid[:, :], in1=dd[:, :], op=ALU.add),
                       reads=[mid.k, dd.k], writes=[hi.k])
            K.emit(Q2, lambda e: e.dma_start(out=S['THR'], in_=lo[:, :]), reads=[lo.k], writes=self.dk['THR'][0], lane=lo.name)

    def phaseF(self, l):
        K, I, S = self.K, self.I, self.Sx
        D, KC = self.D, self.KC
        last = (l == self.L - 1)
        with self.phase():
            hT = Buf(K, 'fhT', [128, KC, TB], BF16)
            hid = Buf(K, 'fhid', [128, 2 * NE, TB], BF16)
            w8 = Ring(K, 'fw8', 8, [128, KC, 128], BF16)
            w32 = Ring(K, 'fw32', 3, [128, 32, 128], BF16)
            sel = Buf(K, 'fsel', [NE, NE, 128], F32)
            thr = Buf(K, 'fthr', [NE, 2], F32)
            affb = Buf(K, 'faff', [NE, TB], F32)
            gm = Buf(K, 'fgm', [NE, TB], F32)
            t32 = Ring(K, 'ft32', 3, [128, TB], F32)
            xr = Ring(K, 'fx', 3, [128, TB], F32)
            ps = self.psum_pool(8)
            K.emit(Q2, lambda e: e.dma_start(out=sel[:, :, :], in_=I['sel']), writes=[sel.k], lane=sel.name)
            K.emit(Q2, lambda e: e.dma_start(out=thr[:, :], in_=S['THR'].rearrange("(e s) c -> e s c", s=8)[:, 0, :]),
                   reads=self.dk['THR'][0], writes=[thr.k], lane=thr.name)
            for bi, (b0, n, s) in enumerate(self.blocks):
                if last and s == 0:
                    continue
                K.emit(Q2, lambda e, b0=b0, n=n: e.dma_start(out=hT[:, :, :n], in_=S['HT'][:, :, b0:b0 + n]),
                       reads=self.dk['HT'][bi], writes=[hT.k], lane=hT.name)
                affs = S['AFFC'][:, 0:n] if s == 0 else S['AFFL'][:, b0 - self.CTX:b0 - self.CTX + n]
                K.emit(Q2, lambda e, affs=affs, n=n: e.dma_start(out=affb[:, :n], in_=affs),
                       reads=self.dk['AFF'][bi], writes=[affb.k], lane=affb.name)
                tc = 1 - s
                K.emit('dve', lambda e, tc=tc: e.scalar_tensor_tensor(
                    out=gm[:, :n], in0=affb[:, :n], scalar=thr[:, tc:tc + 1], in1=affb[:, :n], op0=ALU.is_ge, op1=ALU.mult),
                    reads=[affb.k, thr.k], writes=[gm.k])
                for ex in range(NE):
                    pg = ps.next()
                    K.emit('pe', lambda e, pg=pg, ex=ex: e.matmul(pg[:, :n], sel[:, ex, :], gm[:, :n], start=True, stop=True),
                           reads=[sel.k, gm.k], writes=[pg.k])
                    for fc in range(2):
                        w1 = w8.next()
                        K.emit('sp', lambda e, w1=w1, ex=ex, fc=fc: e.dma_start(out=w1[:, :, :], in_=S['w1'][l][2 * ex + fc]),
                               reads=[self.wk[('w1', l)]], writes=[w1.k], lane=w1.name)
                        w3 = w8.next()
                        K.emit('sp', lambda e, w3=w3, ex=ex, fc=fc: e.dma_start(out=w3[:, :, :], in_=S['w3'][l][2 * ex + fc]),
                               reads=[self.wk[('w3', l)]], writes=[w3.k], lane=w3.name)
                        p1, p3 = ps.next(), ps.next()
                        self.mm_group(p1, p1[:, :n], w1, hT, KC, n)
                        self.mm_group(p3, p3[:, :n], w3, hT, KC, n)
                        tt = t32.next()
                        K.emit('act', lambda e, p1=p1, tt=tt: e.activation(tt[:, :n], p1[:, :n], AF.Silu), reads=[p1.k], writes=[tt.k])
                        K.emit('dve', lambda e, p3=p3, tt=tt: e.tensor_tensor(out=tt[:, :n], in0=tt[:, :n], in1=p3[:, :n], op=ALU.mult),
                               reads=[tt.k, p3.k], writes=[tt.k])
                        K.emit('dve', lambda e, pg=pg, tt=tt, ex=ex, fc=fc: e.tensor_tensor(
                            out=hid[:, 2 * ex + fc, :n], in0=tt[:, :n], in1=pg[:, :n], op=ALU.mult),
                            reads=[tt.k, pg.k], writes=[hid.k])
                for dc in range(KC):
                    w2 = w32.next()
                    K.emit('sp', lambda e, w2=w2, dc=dc: e.dma_start(out=w2[:, :, :], in_=S['w2'][l][dc]),
                           reads=[self.wk[('w2', l)]], writes=[w2.k], lane=w2.name)
                    xb = xr.next()
                    K.emit(Q2, lambda e, xb=xb, dc=dc, b0=b0, n=n: e.dma_start(out=xb[:, :n], in_=self.xt1(dc, b0, b0 + n)),
                           reads=[self.dk['XT'][bi][dc]], writes=[xb.k], lane=xb.name)
                    p = ps.next()
                    self.mm_group(p, p[:, :n], w2, hid, 2 * NE, n)
                    K.emit('dve', lambda e, p=p, xb=xb, dc=dc, s=s: e.scalar_tensor_tensor(
                        out=xb[:, :n], in0=p[:, :n], scalar=self.mod[:, 5, dc, s:s + 1], in1=xb[:, :n], op0=ALU.mult, op1=ALU.add),
                        reads=[p.k, xb.k, self.mod.k], writes=[xb.k])
                    K.emit(Q2, lambda e, xb=xb, dc=dc, b0=b0, n=n: e.dma_start(out=self.xt1(dc, b0, b0 + n), in_=xb[:, :n]),
                           reads=[xb.k], writes=[self.dk['XT'][bi][dc]], lane=xb.name)

    def final(self):
        K, I, S = self.K, self.I, self.Sx
        D, KC, CTX = self.D, self.KC, self.CTX
        self.out_tok = Tok()
        with self.phase():
            xr = Ring(K, 'zx', 2, [128, min(4, KC), TB], F32)
            sqr = Ring(K, 'zsq', 2, [128, TB], BF16)
            rs = Buf(K, 'zrs', [128, TB], F32)
            fg = Buf(K, 'zfg', [128, KC], F32)
            yT = Buf(K, 'zyT', [128, KC, TB], F32)
            ost = Ring(K, 'zost', 2, [128, D], F32)
            ps = self.psum_pool(6)
            K.emit(Q2, lambda e: e.dma_start(out=fg[:, :], in_=I['fg']), writes=[fg.k], lane=fg.name)
            for bi, (b0, n, s) in enumerate(self.blocks):
                if s == 0:
                    continue
                psn = ps.next()
                self.norm_stats(S['XT'], bi, xr, sqr, psn, rs, float(D))
                self.norm_apply(S['XT'], bi, xr, rs, fg, None, yT, s)
                for j in range(n // 128):
                    o = ost.next()
                    for c0 in range(0, KC, 4):
                        p = ps.next()
                        for q in range(4):
                            kc = c0 + q
                            K.emit('pe', lambda e, p=p, q=q, kc=kc, j=j: e.transpose(
                                p[:, q * 128:(q + 1) * 128], yT[:, kc, j * 128:(j + 1) * 128], self.ident[:, :]),
                                reads=[yT.k, self.ident.k], writes=[p.k])
                        if (c0 // 4) % 2:
                            K.emit('act', lambda e, p=p, o=o, c0=c0: e.activation(o[:, c0 * 128:(c0 + 4) * 128], p[:, :], AF.Copy),
                                   reads=[p.k], writes=[o.k])
                        else:
                            K.emit('dve', lambda e, p=p, o=o, c0=c0: e.tensor_copy(o[:, c0 * 128:(c0 + 4) * 128], p[:, :]),
                                   reads=[p.k], writes=[o.k])
                    r0 = b0 - CTX + j * 128
                    K.emit(Q2, lambda e, o=o, r0=r0: e.dma_start(out=self.out[r0:r0 + 128, :], in_=o[:, :]),
                           reads=[o.k], writes=[self.out_tok], lane=o.name)


class _ModView:
    def __init__(self, mod, j):
        self.mod = mod
        self.j = j
        self.k = mod.k

    def __getitem__(self, idx):
        return self.mod.t[:, self.j][idx]


def host_inputs(D, SEQ, CTX, L, x, c, ctx, c_ctx, ada_w, ada_b, norm1_g, norm2_g, w_in, w_dec_up, b_dec,
                gla_norm_g, sg_ln_g, sg_ln_b, sg_w, sg_b, w_branch_a, w_branch_b, w_out,
                w_router, w_exp1, w_exp3, w_exp2, final_g):
    KC = D // 128
    f = lambda a: np.ascontiguousarray(np.asarray(a, dtype=np.float32))
    m = {}
    m['x'] = f(x)[0]
    m['ctx'] = f(ctx)[0]
    cc = np.stack([f(c_ctx), f(c)[0]], axis=-1)
    m['ccT'] = np.ascontiguousarray(cc.reshape(KC, 128, 2).transpose(1, 0, 2))
    m['ada_w'] = f(ada_w)
    m['ada_b'] = np.ascontiguousarray(f(ada_b).reshape(L, 6 * KC, 128).transpose(0, 2, 1))
    m['n1g'] = np.ascontiguousarray(f(norm1_g).reshape(L, KC, 128).transpose(0, 2, 1))
    m['n2g'] = np.ascontiguousarray(f(norm2_g).reshape(L, KC, 128).transpose(0, 2, 1))
    m['fg'] = np.ascontiguousarray(f(final_g).reshape(KC, 128).T)
    m['w_in'] = f(w_in)
    wup = np.zeros((L, 2, 33, 512), np.float32)
    wup[:, 0, 0:16] = f(w_dec_up)[:, 0]
    wup[:, 1, 16:32] = f(w_dec_up)[:, 1]
    wup[:, :, 32] = f(b_dec)
    m['wup'] = wup
    m['glag'] = np.ascontiguousarray(f(gla_norm_g).reshape(L, NH, 128).transpose(0, 2, 1))
    m['lng'] = f(sg_ln_g)
    m['lnb'] = f(sg_ln_b)
    m['wsT'] = np.ascontiguousarray(f(sg_w).transpose(0, 3, 1, 2))
    m['sgb'] = np.ascontiguousarray(f(sg_b).reshape(L, 1024))
    m['wa'] = f(w_branch_a)
    m['wb'] = f(w_branch_b)
    m['wo'] = f(w_out)
    m['wr'] = f(w_router)
    m['w1'] = f(w_exp1)
    m['w3'] = f(w_exp3)
    m['w2'] = np.ascontiguousarray(f(w_exp2).reshape(L, NE * FF, D))
    m['ident'] = np.eye(128, dtype=np.float32)
    jj, ii = np.meshgrid(np.arange(128), np.arange(128), indexing='ij')
    m['maskF'] = (jj <= ii).astype(np.float32)
    m['maskB'] = (jj >= ii).astype(np.float32)
    m['bsel'] = ((jj // 8) == (ii // 8)).astype(np.float32)
    sel = np.zeros((NE, NE, 128), np.float32)
    for e in range(NE):
        sel[e, e, :] = 1.0
    m['sel'] = sel
    return m


_CACHE = {}


def run(D, SEQ, CTX, L, inputs, debug=False):
    key = (D, SEQ, CTX, L, debug)
    if key not in _CACHE:
        _CACHE[key] = Builder(D, SEQ, CTX, L, debug).build()
    nc = _CACHE[key]
    m = host_inputs(D, SEQ, CTX, L, **inputs)
    res = run_bass_kernel_spmd(nc, [m], core_ids=[0])
    if debug:
        return res.results[0]
    return res.results[0]['out'].reshape(1, SEQ, D).astype(np.float32)


def kernel(**inputs):
    return run(4096, 16384, 256, 4, inputs)
```

```python
import numpy as np
from contextlib import ExitStack
import concourse.bass as bass
import concourse.mybir as mybir
from concourse.bass_utils import run_bass_kernel_spmd

F32 = mybir.dt.float32
BF16 = mybir.dt.bfloat16
AF = mybir.ActivationFunctionType
ALU = mybir.AluOpType
AX = mybir.AxisListType

EPS = 1e-6
NH = 8
DK = 64
NE = 16
FF = 256
TB = 512
NBIS = 34
Q2 = 'pool'


class Tok:
    __slots__ = ('w', 'r')

    def __init__(self):
        self.w = None
        self.r = []


class Sched:
    def __init__(self, nc, stack):
        self.nc = nc
        self.stack = stack
        self.E = {'pe': nc.tensor, 'act': nc.scalar, 'dve': nc.vector, 'pool': nc.gpsimd, 'sp': nc.sync}
        self.sem = {e: stack.enter_context(nc.semaphore('s_' + e)) for e in self.E}
        self.cnt = {e: 0 for e in self.E}
        self.seen = {e: {} for e in self.E}
        self.lanes = {}
        self.multi_lanes = set()
        self.n_inst = 0

    def lane(self, name):
        if name not in self.lanes:
            self.lanes[name] = [self.stack.enter_context(self.nc.semaphore('d_' + name)), 0]
        return self.lanes[name]

    def _semof(self, key):
        if key in self.sem:
            return self.sem[key]
        return self.lanes[key][0]

    def emit(self, eng, fn, reads=(), writes=(), lane=None, multi=False, defer=False, skip_waw=False):
        deps = {}
        if multi:
            self.multi_lanes.add(lane)
        if lane is not None and not multi:
            ln = self.lane(lane)
            if ln[1] > 0:
                deps[lane] = ln[1]
        for t in reads:
            if t.w is not None:
                k, v = t.w
                if deps.get(k, 0) < v:
                    deps[k] = v
        for t in writes:
            if t.w is not None and not skip_waw:
                k, v = t.w
                if deps.get(k, 0) < v:
                    deps[k] = v
            for (k, v) in t.r:
                if deps.get(k, 0) < v:
                    deps[k] = v
        seen = self.seen[eng]
        e = self.E[eng]
        for k, v in deps.items():
            if eng == 'pe' and k == 'pe':
                continue
            if seen.get(k, 0) < v:
                seen[k] = v
                e.wait_ge(self._semof(k), v)
                self.n_inst += 1
        ins = fn(e)
        if lane is not None:
            ln = self.lane(lane)
            ln[1] += 16
            ticket = (lane, ln[1])
            ins.then_inc(ln[0], 16)
        elif defer:
            ticket = (eng, self.cnt[eng] + 1)
        else:
            self.cnt[eng] += 1
            ticket = (eng, self.cnt[eng])
            ins.then_inc(self.sem[eng], 1)
        for t in reads:
            if len(t.r) > 6:
                best = {}
                for (k, v) in t.r:
                    if best.get(k, 0) < v:
                        best[k] = v
                t.r = list(best.items())
            t.r.append(ticket)
        for t in writes:
            t.w = ticket
            t.r = []
        self.n_inst += 1
        return ticket

    def barrier(self):
        for eng, e in self.E.items():
            seen = self.seen[eng]
            for k, c in self.cnt.items():
                if k != eng and c > 0 and seen.get(k, 0) < c:
                    seen[k] = c
                    e.wait_ge(self.sem[k], c)
                    self.n_inst += 1
            for nm, (sem, c) in self.lanes.items():
                if nm in self.multi_lanes:
                    continue
                if c > 0 and seen.get(nm, 0) < c:
                    seen[nm] = c
                    e.wait_ge(sem, c)
                    self.n_inst += 1

    def wait_all(self, eng, toks):
        seen = self.seen[eng]
        e = self.E[eng]
        for t in toks:
            if t.w is not None:
                k, v = t.w
                if seen.get(k, 0) < v:
                    seen[k] = v
                    e.wait_ge(self._semof(k), v)


class Buf:
    def __init__(self, K, name, shape, dtype, psum=False):
        nc = K.nc
        K.uid = getattr(K, 'uid', 0) + 1
        tname = f"t{K.uid}_{name}"
        if psum:
            self.t = K.cur.enter_context(nc.psum_tensor(tname, shape, dtype))
        else:
            self.t = K.cur.enter_context(nc.sbuf_tensor(tname, shape, dtype))
        self.k = Tok()
        if psum:
            self.name = name
        else:
            self.name = f"L{K.lane_ctr}"
            K.lane_ctr += 1

    def __getitem__(self, idx):
        return self.t[idx]


class Ring:
    def __init__(self, K, name, n, shape, dtype, psum=False):
        self.b = [Buf(K, f"{name}{i}", shape, dtype, psum) for i in range(n)]
        self.i = 0

    def next(self):
        b = self.b[self.i % len(self.b)]
        self.i += 1
        return b


class Builder:
    def __init__(self, D, SEQ, CTX, L, debug=False):
        self.debug = debug
        self.D, self.SEQ, self.CTX, self.L = D, SEQ, CTX, L
        self.KC = D // 128
        self.T = CTX + SEQ
        self.NIN = 5152 + 2 * D
        self.OFF_GA = 5152
        self.OFF_GB = 5152 + D
        self.blocks = [(0, CTX, 0)] + [(CTX + TB * i, TB, 1) for i in range(SEQ // TB)]
        self.NT = self.T // 128
        assert SEQ % TB == 0 and CTX % 128 == 0 and CTX <= TB

    def declare(self):
        nc = self.nc
        D, SEQ, CTX, L, KC, T, NIN = self.D, self.SEQ, self.CTX, self.L, self.KC, self.T, self.NIN

        def inp(name, shape, dt=F32):
            return nc.dram_tensor(name, list(shape), dt, kind="ExternalInput").ap()

        def scr(name, shape, dt):
            return nc.dram_tensor(name, list(shape), dt, kind="Internal").ap()

        I = {}
        I['x'] = inp('x', [SEQ, D])
        I['ctx'] = inp('ctx', [CTX, D])
        I['ccT'] = inp('ccT', [128, KC, 2])
        I['ada_w'] = inp('ada_w', [L, D, 6 * D])
        I['ada_b'] = inp('ada_b', [L, 128, 6 * KC])
        I['n1g'] = inp('n1g', [L, 128, KC])
        I['n2g'] = inp('n2g', [L, 128, KC])
        I['fg'] = inp('fg', [128, KC])
        I['w_in'] = inp('w_in', [L, D, NIN])
        I['wup'] = inp('wup', [L, 2, 33, 512])
        I['glag'] = inp('glag', [L, 128, NH])
        I['lng'] = inp('lng', [L, 1024])
        I['lnb'] = inp('lnb', [L, 1024])
        I['wsT'] = inp('wsT', [L, 128, NH, 128])
        I['sgb'] = inp('sgb', [L, 1024])
        I['wa'] = inp('wa', [L, 1024, D])
        I['wb'] = inp('wb', [L, 1024, D])
        I['wo'] = inp('wo', [L, D, D])
        I['wr'] = inp('wr', [L, D, NE])
        I['w1'] = inp('w1', [L, NE, D, FF])
        I['w3'] = inp('w3', [L, NE, D, FF])
        I['w2'] = inp('w2', [L, NE * FF, D])
        I['ident'] = inp('ident', [128, 128])
        I['maskF'] = inp('maskF', [128, 128])
        I['maskB'] = inp('maskB', [128, 128])
        I['bsel'] = inp('bsel', [128, 128])
        I['sel'] = inp('sel', [NE, NE, 128])
        self.I = I
        self.out = nc.dram_tensor('out', [SEQ, D], F32, kind="ExternalOutput").ap()

        S = {}
        self.NCT_A = 41
        S['win'] = [scr(f's_win{l}', [41 + 2 * KC, 128, KC, 128], BF16) for l in range(L)]
        S['wa'] = [scr(f's_wa{l}', [KC, 128, 8, 128], BF16) for l in range(L)]
        S['wb'] = [scr(f's_wb{l}', [KC, 128, 8, 128], BF16) for l in range(L)]
        S['wo'] = [scr(f's_wo{l}', [KC, 128, KC, 128], BF16) for l in range(L)]
        S['wr'] = [scr(f's_wr{l}', [128, KC, NE], BF16) for l in range(L)]
        S['w1'] = [scr(f's_w1{l}', [NE * 2, 128, KC, 128], BF16) for l in range(L)]
        S['w3'] = [scr(f's_w3{l}', [NE * 2, 128, KC, 128], BF16) for l in range(L)]
        S['w2'] = [scr(f's_w2{l}', [KC, 128, 32, 128], BF16) for l in range(L)]
        S['wsT'] = [scr(f's_wsT{l}', [128, NH, 128], BF16) for l in range(L)]
        self.KCP = KC if 128 * KC * T * 4 <= 200 * 2 ** 20 else KC // 2
        self.xt_parts = [scr(f's_XT{i}', [128, self.KCP, T], F32) for i in range(KC // self.KCP)]
        S['XT'] = self.xt_parts[0]
        S['HT'] = scr('s_HT', [128, KC, T], BF16)
        S['QK'] = scr('s_QK', [64, 16, T], F32)
        S['ZL'] = scr('s_ZL', [33, T], F32)
        S['RS'] = scr('s_RS', [128, NH, T], F32)
        S['UG'] = scr('s_UG', [128, NH, T], F32)
        S['V'] = scr('s_V', [T, 1024], BF16)
        S['VSN'] = scr('s_VSN', [T, 1024], BF16)
        S['OT'] = scr('s_OT', [128, NH, T], F32)
        S['AFFL'] = scr('s_AFFL', [NE, SEQ], F32)
        S['AFFC'] = scr('s_AFFC', [NE, CTX], F32)
        S['THR'] = scr('s_THR', [128, 2], F32)
        self.Sx = S
        nb = len(self.blocks)
        nsub = {'XT': KC, 'HT': 1, 'QK': 8, 'ZL': 2, 'RS': NH, 'UG': NH, 'V': TB // 128, 'VSN': TB // 128, 'OT': 1, 'AFF': 1}
        self.dk = {name: [[Tok() for _ in range(ns)] for _ in range(nb)] for name, ns in nsub.items()}
        self.dk['THR'] = [[Tok()]]
        self.wk = {}
        for l in range(L):
            for name in ['win', 'winG', 'wa', 'wb', 'wo', 'wr', 'w1', 'w3', 'w2', 'wsT']:
                self.wk[(name, l)] = Tok()

    def coltiles_A(self):
        tiles = []
        for i in range(4):
            tiles.append(('q', i, 128 * i, 128))
        for i in range(4):
            tiles.append(('k', i, 512 + 128 * i, 128))
        for i in range(8):
            tiles.append(('v', i, 1024 + 128 * i, 128))
        tiles.append(('z', 0, 2048, 32))
        for i in range(8):
            tiles.append(('r', i, 2080 + 128 * i, 128))
        for i in range(8):
            tiles.append(('u', i, 3104 + 128 * i, 128))
        for i in range(8):
            tiles.append(('s', i, 4128 + 128 * i, 128))
        return tiles

    def emit_casts(self, l):
        K, I, S = self.K, self.I, self.Sx
        KC = self.KC

        def cast(dst, src, tok, lane):
            if len(dst.shape) == 4:
                for g in range(dst.shape[0]):
                    K.emit('pool', lambda e, g=g: e.dma_start(out=dst[g], in_=src[g]), writes=[tok], lane=lane, multi=True, skip_waw=True)
            else:
                K.emit('pool', lambda e: e.dma_start(out=dst, in_=src), writes=[tok], lane=lane, multi=True, skip_waw=True)

        win = I['w_in'][l]
        tk = self.wk[('win', l)]
        ln = f'cw{l}'
        for g0 in range(0, 16, 4):
            cast(S['win'][l][g0:g0 + 4],
                 win[:, 128 * g0:128 * (g0 + 4)].rearrange("(kc p) (g c) -> g p kc c", p=128, c=128), tk, ln)
        cast(S['win'][l][16, :, :, 0:32], win[:, 2048:2080].rearrange("(kc p) c -> p kc c", p=128), tk, ln)
        for g0 in range(0, 24, 4):
            c0 = 2080 + 128 * g0
            cast(S['win'][l][17 + g0:17 + g0 + 4],
                 win[:, c0:c0 + 512].rearrange("(kc p) (g c) -> g p kc c", p=128, c=128), tk, ln)
        tk = self.wk[('winG', l)]
        ln = f'cg{l}'
        for g0 in range(0, 2 * KC, 4):
            c0 = 5152 + 128 * g0
            cast(S['win'][l][41 + g0:41 + g0 + 4],
                 win[:, c0:c0 + 512].rearrange("(kc p) (g c) -> g p kc c", p=128, c=128), tk, ln)
        for nm in ['wa', 'wb', 'wo']:
            tk = self.wk[(nm, l)]
            for g0 in range(0, KC, 4):
                cast(S[nm][l][g0:g0 + 4],
                     I[nm][l][:, 128 * g0:128 * (g0 + 4)].rearrange("(kc p) (g c) -> g p kc c", p=128, c=128),
                     tk, f'c{nm}{l}')
        cast(S['wr'][l], I['wr'][l].rearrange("(kc p) c -> p kc c", p=128), self.wk[('wr', l)], f'cwr{l}')
        for nm in ['w1', 'w3']:
            tk = self.wk[(nm, l)]
            for e0 in range(0, NE, 2):
                for e in range(e0, e0 + 2):
                    cast(S[nm][l][2 * e:2 * e + 2],
                         I[nm][l, e].rearrange("(kc p) (g c) -> g p kc c", p=128, c=128), tk, f'c{nm}{l}')
        tk = self.wk[('w2', l)]
        for g0 in range(0, KC, 4):
            cast(S['w2'][l][g0:g0 + 4],
                 I['w2'][l][:, 128 * g0:128 * (g0 + 4)].rearrange("(kc p) (g c) -> g p kc c", p=128, c=128),
                 tk, f'cw2{l}')
        cast(S['wsT'][l], I['wsT'][l], self.wk[('wsT', l)], f'cws{l}')

    def xt(self, k0, k1, t0, t1):
        p = k0 // self.KCP
        assert (k1 - 1) // self.KCP == p
        return self.xt_parts[p][:, k0 - p * self.KCP:k1 - p * self.KCP, t0:t1]

    def xt1(self, kc, t0, t1):
        p = kc // self.KCP
        return self.xt_parts[p][:, kc - p * self.KCP, t0:t1]

    def dump(self, name, src, toks):
        if not self.debug:
            return
        dst = self.nc.dram_tensor('dbg_' + name, list(src.shape), src.dtype, kind="ExternalOutput").ap()
        flat = []
        for t in toks:
            if isinstance(t, list):
                for u in t:
                    flat.extend(u if isinstance(u, list) else [u])
            else:
                flat.append(t)
        self.K.emit(Q2, lambda e: e.dma_start(out=dst, in_=src), reads=flat, writes=[Tok()], lane='dbg')

    def phase(self):
        class P:
            def __init__(s, B):
                s.B = B

            def __enter__(s):
                s.st = ExitStack()
                s.st.__enter__()
                s.B.K.cur = s.st
                s.B.K.lane_ctr = s.B.K.lane_base
                s.B.K.barrier()
                return s

            def __exit__(s, *a):
                s.B.K.cur = s.B.K.stack
                return s.st.__exit__(*a)
        return P(self)

    def psum_pool(self, n=8):
        return Ring(self.K, 'ps', n, [128, 512], F32, psum=True)

    def blk_of_tile(self, ti):
        t0 = ti * 128
        for bi, (b0, n, s) in enumerate(self.blocks):
            if b0 <= t0 < b0 + n:
                return bi
        raise AssertionError

    def mm_group(self, ps, pout, wt, act, nk, n, M=128, w0=0):
        K = self.K
        for kc in range(nk):
            K.emit('pe', lambda e, kc=kc: e.matmul(pout, wt[:, kc, w0:w0 + M], act[:, kc, :n],
                                                   start=(kc == 0), stop=(kc == nk - 1)),
                   reads=[wt.k, act.k], writes=[ps.k], defer=(kc < nk - 1))

    def norm_stats(self, XTsrc, bi, xr, sqr, ps, rs, scale_div):
        K = self.K
        b0, n, s = self.blocks[bi]
        KC = self.KC
        G = min(4, KC)
        first = True
        for g0 in range(0, KC, G):
            xb = xr.next()
            K.emit(Q2, lambda e, xb=xb, g0=g0: e.dma_start(out=xb[:, 0:G, :n], in_=self.xt(g0, g0 + G, b0, b0 + n)),
                   reads=self.dk['XT'][bi][g0:g0 + G], writes=[xb.k], lane=xb.name)
            for j in range(G):
                sq = sqr.next()
                K.emit('act', lambda e, xb=xb, sq=sq, j=j: e.activation(sq[:, :n], xb[:, j, :n], AF.Square),
                       reads=[xb.k], writes=[sq.k])
                last = (g0 + j == KC - 1)
                K.emit('pe', lambda e, sq=sq, first=first, last=last: e.matmul(
                    ps[:, :n], self.ones_bf[:, :], sq[:, :n], start=first, stop=last),
                    reads=[sq.k, self.ones_bf.k], writes=[ps.k])
                first = False
        K.emit('act', lambda e: e.activation(rs[:, :n], ps[:, :n], AF.Sqrt, bias=self.eps_t[:, 0:1], scale=1.0 / scale_div),
               reads=[ps.k, self.eps_t.k], writes=[rs.k])
        K.emit('dve', lambda e: e.reciprocal(rs[:, :n], rs[:, :n]), reads=[rs.k], writes=[rs.k])

    def norm_apply(self, XTsrc, bi, xr, rs, A, Bv, hT, s):
        K = self.K
        b0, n, _ = self.blocks[bi]
        KC = self.KC
        G = min(4, KC)
        for g0 in range(0, KC, G):
            xb = xr.next()
            K.emit(Q2, lambda e, xb=xb, g0=g0: e.dma_start(out=xb[:, 0:G, :n], in_=self.xt(g0, g0 + G, b0, b0 + n)),
                   reads=self.dk['XT'][bi][g0:g0 + G], writes=[xb.k], lane=xb.name)
            K.emit('dve', lambda e, xb=xb: e.tensor_tensor(
                out=xb[:, 0:G, :n], in0=xb[:, 0:G, :n],
                in1=rs[:, :n].unsqueeze(1).to_broadcast([128, G, n]), op=ALU.mult),
                reads=[xb.k, rs.k], writes=[xb.k])
            for j in range(G):
                kc = g0 + j
                if Bv is None:
                    K.emit('act', lambda e, xb=xb, j=j, kc=kc: e.activation(
                        hT[:, kc, :n], xb[:, j, :n], AF.Identity, scale=A[:, kc:kc + 1]),
                        reads=[xb.k, A.k], writes=[hT.k])
                elif kc % 2 == 0:
                    K.emit('act', lambda e, xb=xb, j=j, kc=kc: e.activation(
                        hT[:, kc, :n], xb[:, j, :n], AF.Identity,
                        bias=Bv[:, kc, s:s + 1], scale=A[:, kc, s:s + 1]),
                        reads=[xb.k, A.k, Bv.k], writes=[hT.k])
                else:
                    K.emit('dve', lambda e, xb=xb, j=j, kc=kc: e.tensor_scalar(
                        out=hT[:, kc, :n], in0=xb[:, j, :n], scalar1=A[:, kc, s:s + 1],
                        scalar2=Bv[:, kc, s:s + 1], op0=ALU.mult, op1=ALU.add),
                        reads=[xb.k, A.k, Bv.k], writes=[hT.k])

    def gelu_evac(self, src_ps, src_tok, dst, dst_tok, tmpa, tmpb, n, P=128):
        K = self.K
        K.emit('act', lambda e: e.activation(tmpa[:P, :n], src_ps, AF.Copy), reads=[src_tok], writes=[tmpa.k])
        K.emit('dve', lambda e: e.tensor_tensor(out=tmpb[:P, :n], in0=tmpa[:P, :n], in1=tmpa[:P, :n], op=ALU.mult),
               reads=[tmpa.k], writes=[tmpb.k])
        K.emit('dve', lambda e: e.tensor_scalar(out=tmpb[:P, :n], in0=tmpb[:P, :n], scalar1=0.044715, scalar2=1.0,
                                                op0=ALU.mult, op1=ALU.add), reads=[tmpb.k], writes=[tmpb.k])
        K.emit('dve', lambda e: e.tensor_tensor(out=tmpb[:P, :n], in0=tmpb[:P, :n], in1=tmpa[:P, :n], op=ALU.mult),
               reads=[tmpb.k, tmpa.k], writes=[tmpb.k])
        K.emit('act', lambda e: e.activation(tmpb[:P, :n], tmpb[:P, :n], AF.Sigmoid, scale=1.5957691216057308),
               reads=[tmpb.k], writes=[tmpb.k])
        K.emit('dve', lambda e: e.tensor_tensor(out=dst, in0=tmpa[:P, :n], in1=tmpb[:P, :n], op=ALU.mult),
               reads=[tmpa.k, tmpb.k], writes=[dst_tok])

    def build(self):
        nc = bass.Bass("TRN2", target_bir_lowering=False)
        self.nc = nc
        self.declare()
        with ExitStack() as st:
            K = Sched(nc, st)
            K.stack = st
            K.cur = st
            K.lane_ctr = 0
            self.K = K
            self.consts()
            K.lane_base = K.lane_ctr
            self.emit_casts(0)
            self.prologue()
            S, dk = self.Sx, self.dk
            self.dump('XT0', S['XT'], dk['XT'])
            upto = getattr(self, 'upto', 99)
            for l in range(self.L):
                if upto < 1:
                    break
                self.adaln(l)
                if l + 1 < self.L:
                    self.emit_casts(l + 1)
                if l == 0:
                    self.dump('MOD', self.mod[:, :, :, :], [self.mod.k])
                if upto < 2:
                    break
                self.phaseA(l)
                if upto < 3:
                    break
                if l == 0:
                    for nm in ['HT', 'QK', 'ZL', 'RS', 'UG', 'V', 'VSN']:
                        self.dump(nm, S[nm], dk[nm])
                self.phaseB(l)
                if l == 0:
                    self.dump('OT', S['OT'], dk['OT'])
                if upto < 4:
                    break
                self.phaseC(l)
                if l == 0:
                    self.dump('XT1', S['XT'], dk['XT'])
                if upto < 5:
                    break
                self.phaseD(l)
                if l == 0:
                    self.dump('H2T', S['HT'], dk['HT'])
                    self.dump('AFFL', S['AFFL'], dk['AFF'])
                    self.dump('AFFC', S['AFFC'], dk['AFF'])
                if upto < 6:
                    break
                self.phaseE(l)
                if l == 0:
                    self.dump('THR', S['THR'], dk['THR'])
                if upto < 7:
                    break
                self.phaseF(l)
                if l == 0:
                    self.dump('XT2', S['XT'], dk['XT'])
            if upto >= 99:
                self.final()
            for nm, (sem, cnt) in K.lanes.items():
                if cnt > 0:
                    nc.sync.wait_ge(sem, cnt)
        return nc

    def consts(self):
        K, I = self.K, self.I
        KC = self.KC
        self.ident = Buf(K, 'ident', [128, 128], F32)
        self.maskF = Buf(K, 'maskF', [128, 128], F32)
        self.maskB = Buf(K, 'maskB', [128, 128], F32)
        self.ones_bf = Buf(K, 'ones_bf', [128, 128], BF16)
        self.ones32 = Buf(K, 'ones32', [128, 128], F32)
        self.eps_t = Buf(K, 'eps_t', [128, 1], F32)
        self.mod = Buf(K, 'mod', [128, 6, KC, 2], F32)
        self.A1 = Buf(K, 'A1', [128, KC, 2], F32)
        self.A2 = Buf(K, 'A2', [128, KC, 2], F32)
        self.cc = Buf(K, 'cc', [128, KC, 2], F32)
        self.scT = Buf(K, 'scT', [128, KC, 2], BF16)
        for b, nm in [(self.ident, 'ident'), (self.maskF, 'maskF'), (self.maskB, 'maskB')]:
            K.emit(Q2, lambda e, b=b, nm=nm: e.dma_start(out=b[:, :], in_=I[nm]), writes=[b.k], lane=b.name)
        K.emit('dve', lambda e: e.memset(self.ones_bf[:, :], 1.0), writes=[self.ones_bf.k])
        K.emit('dve', lambda e: e.memset(self.ones32[:, :], 1.0), writes=[self.ones32.k])
        K.emit('dve', lambda e: e.memset(self.eps_t[:, :], EPS), writes=[self.eps_t.k])
        K.emit(Q2, lambda e: e.dma_start(out=self.cc[:, :, :], in_=I['ccT']), writes=[self.cc.k], lane=self.cc.name)
        K.emit('act', lambda e: e.activation(self.scT[:, :, :], self.cc[:, :, :], AF.Silu),
               reads=[self.cc.k], writes=[self.scT.k])
        self.onesrow = Buf(K, 'onesrow', [1, TB], F32)
        K.emit('dve', lambda e: e.memset(self.onesrow[:, :], 1.0), writes=[self.onesrow.k])
        T = self.T
        for bi, (b0, n, s) in enumerate(self.blocks):
            K.emit(Q2, lambda e, b0=b0, n=n: e.dma_start(out=self.Sx['ZL'][32:33, b0:b0 + n], in_=self.onesrow[0:1, 0:n]),
                   reads=[self.onesrow.k], writes=[self.dk['ZL'][bi][1]], lane=self.onesrow.name)

    def prologue(self):
        K, I = self.K, self.I
        D, KC, CTX = self.D, self.KC, self.CTX
        with self.phase():
            xin = Ring(K, 'pxin', 2, [128, D], F32)
            stg = Ring(K, 'pstg', 2, [128, KC, 128], F32)
            ps = self.psum_pool(4)
            for ti in range(self.NT):
                t0 = ti * 128
                src = I['ctx'][t0:t0 + 128, :] if t0 < CTX else I['x'][t0 - CTX:t0 - CTX + 128, :]
                xb = xin.next()
                K.emit(Q2, lambda e, xb=xb, src=src: e.dma_start(out=xb[:, :], in_=src), writes=[xb.k], lane=xb.name)
                sg = stg.next()
                for c0 in range(0, KC, 4):
                    p = ps.next()
                    for j in range(4):
                        kc = c0 + j
                        K.emit('pe', lambda e, p=p, xb=xb, j=j, kc=kc: e.transpose(
                            p[:, j * 128:(j + 1) * 128], xb[:, kc * 128:(kc + 1) * 128], self.ident[:, :]),
                            reads=[xb.k, self.ident.k], writes=[p.k])
                    eng = 'act' if (c0 // 4) % 2 else 'dve'
                    if eng == 'act':
                        K.emit('act', lambda e, p=p, sg=sg, c0=c0: e.activation(
                            sg[:, c0:c0 + 4, :], p[:, :].rearrange("p (j t) -> p j t", j=4), AF.Copy),
                            reads=[p.k], writes=[sg.k])
                    else:
                        K.emit('dve', lambda e, p=p, sg=sg, c0=c0: e.tensor_copy(
                            sg[:, c0:c0 + 4, :], p[:, :].rearrange("p (j t) -> p j t", j=4)),
                            reads=[p.k], writes=[sg.k])
                bi = self.blk_of_tile(ti)
                for k0 in range(0, KC, self.KCP):
                    K.emit(Q2, lambda e, sg=sg, t0=t0, k0=k0: e.dma_start(
                        out=self.xt(k0, k0 + self.KCP, t0, t0 + 128), in_=sg[:, k0:k0 + self.KCP, :]),
                        reads=[sg.k], writes=self.dk['XT'][bi][k0:k0 + self.KCP], lane=sg.name)

    def adaln(self, l):
        K, I = self.K, self.I
        D, KC = self.D, self.KC
        with self.phase():
            wr = Ring(K, 'adw', 2, [128, KC, 512], BF16)
            adb = Buf(K, 'adb', [128, 6 * KC], F32)
            g1 = Buf(K, 'g1t', [128, KC], F32)
            g2 = Buf(K, 'g2t', [128, KC], F32)
            ps = Buf(K, 'psada', [128, 512], F32, psum=True)
            K.emit(Q2, lambda e: e.dma_start(out=adb[:, :], in_=I['ada_b'][l]), writes=[adb.k], lane=adb.name)
            K.emit(Q2, lambda e: e.dma_start(out=g1[:, :], in_=I['n1g'][l]), writes=[g1.k], lane=g1.name)
            K.emit(Q2, lambda e: e.dma_start(out=g2[:, :], in_=I['n2g'][l]), writes=[g2.k], lane=g2.name)
            ncg = 6 * D // 512
            for cg in range(ncg):
                w = wr.next()
                K.emit('pool', lambda e, w=w, cg=cg: e.dma_start(
                    out=w[:, :, :], in_=I['ada_w'][l][:, cg * 512:(cg + 1) * 512].rearrange("(kc p) c -> p kc c", p=128)),
                    writes=[w.k], lane=w.name)
                for q in range(4):
                    col = cg * 4 + q
                    for kc in range(KC):
                        K.emit('pe', lambda e, w=w, q=q, kc=kc, col=col: e.matmul(
                            ps[:, 2 * col:2 * col + 2], w[:, kc, q * 128:(q + 1) * 128], self.scT[:, kc, :],
                            start=(kc == 0), stop=(kc == KC - 1)),
                            reads=[w.k, self.scT.k], writes=[ps.k])
            K.emit('dve', lambda e: e.tensor_tensor(
                out=self.mod[:, :, :, :].rearrange("p j k s -> p (j k) s"),
                in0=ps[:, 0:12 * KC].rearrange("p (c s) -> p c s", s=2),
                in1=adb[:, :].unsqueeze(2).to_broadcast([128, 6 * KC, 2]), op=ALU.add),
                reads=[ps.k, adb.k], writes=[self.mod.k])
            for (A, g, j) in [(self.A1, g1, 1), (self.A2, g2, 4)]:
                K.emit('dve', lambda e, A=A, j=j: e.tensor_scalar(
                    out=A[:, :, :], in0=self.mod[:, j, :, :], scalar1=1.0, scalar2=None, op0=ALU.add),
                    reads=[self.mod.k], writes=[A.k])
                K.emit('dve', lambda e, A=A, g=g: e.tensor_tensor(
                    out=A[:, :, :], in0=A[:, :, :], in1=g[:, :].unsqueeze(2).to_broadcast([128, KC, 2]), op=ALU.mult),
                    reads=[A.k, g.k], writes=[A.k])

    def phaseA(self, l):
        K, I, S = self.K, self.I, self.Sx
        D, KC = self.D, self.KC
        last = (l == self.L - 1)
        tiles = self.coltiles_A()
        with self.phase():
            xr = Ring(K, 'ax', 2, [128, min(4, KC), TB], F32)
            sqr = Ring(K, 'asq', 2, [128, TB], BF16)
            rs = Buf(K, 'ars', [128, TB], F32)
            hT = Buf(K, 'ahT', [128, KC, TB], BF16)
            wring = Ring(K, 'aw', 6, [128, KC, 128], BF16)
            ps = self.psum_pool(8)
            qkst = Ring(K, 'aqk', 2, [128, TB], F32)
            st32 = Ring(K, 'ast', 3, [128, TB], F32)
            tmpa = Buf(K, 'atma', [128, TB], F32)
            tmpb = Buf(K, 'atmb', [128, TB], F32)
            vT = Buf(K, 'avT', [128, NH, TB], F32)
            gS = Buf(K, 'agS', [128, NH, TB], F32)
            vtok = Ring(K, 'avtok', 2, [128, 1024], BF16)
            lnt = Ring(K, 'alnt', 2, [128, 1024], F32)
            vsn = Ring(K, 'avsn', 2, [128, 1024], BF16)
            lng = Buf(K, 'alng', [128, 1024], F32)
            lnb = Buf(K, 'alnb', [128, 1024], F32)
            stat = Buf(K, 'astat', [128, 2, 6], F32)
            mv = Buf(K, 'amv', [128, 2], F32)
            zst = Buf(K, 'azst', [32, TB], F32)
            K.emit(Q2, lambda e: e.dma_start(out=lng[:, :], in_=I['lng'][l].partition_broadcast(128)),
                   writes=[lng.k], lane=lng.name)
            K.emit(Q2, lambda e: e.dma_start(out=lnb[:, :], in_=I['lnb'][l].partition_broadcast(128)),
                   writes=[lnb.k], lane=lnb.name)
            wtok = self.wk[('win', l)]
            for bi, (b0, n, s) in enumerate(self.blocks):
                psn = ps.next()
                self.norm_stats(S['XT'], bi, xr, sqr, psn, rs, float(D))
                self.norm_apply(S['XT'], bi, xr, rs, self.A1, _ModView(self.mod, 0), hT, s)
                if not (last and s == 0):
                    K.emit(Q2, lambda e, b0=b0, n=n: e.dma_start(out=S['HT'][:, :, b0:b0 + n], in_=hT[:, :, :n]),
                           reads=[hT.k], writes=self.dk['HT'][bi], lane=hT.name)
                for ti, (kind, idx, c0, w) in enumerate(tiles):
                    if last and s == 0 and kind in ('r', 'u', 's'):
                        continue
                    wt = wring.next()
                    K.emit('sp', lambda e, wt=wt, ti=ti, w=w: e.dma_start(out=wt[:, :, 0:w], in_=S['win'][l][ti, :, :, 0:w]),
                           reads=[wtok], writes=[wt.k], lane=wt.name)
                    if kind in ('q', 'k'):
                        qs = qkst.next()
                        p = ps.next()
                        self.mm_group(p, p[:, :n], wt, hT, KC, n)
                        if idx % 2:
                            K.emit('act', lambda e, p=p, qs=qs: e.activation(qs[:, :n], p[:, :n], AF.Copy),
                                   reads=[p.k], writes=[qs.k])
                        else:
                            K.emit('dve', lambda e, p=p, qs=qs: e.tensor_copy(qs[:, :n], p[:, :n]),
                                   reads=[p.k], writes=[qs.k])
                        hh = (0 if kind == 'q' else 8) + 2 * idx
                        for half in range(2):
                            K.emit(Q2, lambda e, qs=qs, hh=hh, half=half, b0=b0, n=n: e.dma_start(
                                out=S['QK'][:, hh + half, b0:b0 + n], in_=qs[64 * half:64 * half + 64, :n]),
                                reads=[qs.k], writes=[self.dk['QK'][bi][hh // 2]], lane=qs.name)
                    elif kind == 'z':
                        p = ps.next()
                        self.mm_group(p, p[0:32, :n], wt, hT, KC, n, M=32, w0=0)
                        K.emit('dve', lambda e, p=p: e.tensor_copy(zst[:, :n], p[0:32, :n]), reads=[p.k], writes=[zst.k])
                        K.emit(Q2, lambda e, b0=b0, n=n: e.dma_start(out=S['ZL'][0:32, b0:b0 + n], in_=zst[:, :n]),
                               reads=[zst.k], writes=[self.dk['ZL'][bi][0]], lane=zst.name)
                    elif kind == 'v':
                        p = ps.next()
                        self.mm_group(p, p[:, :n], wt, hT, KC, n)
                        if idx % 2:
                            K.emit('act', lambda e, p=p, idx=idx: e.activation(vT[:, idx, :n], p[:, :n], AF.Copy),
                                   reads=[p.k], writes=[vT.k])
                        else:
                            K.emit('dve', lambda e, p=p, idx=idx: e.tensor_copy(vT[:, idx, :n], p[:, :n]),
                                   reads=[p.k], writes=[vT.k])
                        if idx == NH - 1:
                            for j in range(n // 128):
                                vt = vtok.next()
                                for hp in range(2):
                                    p2 = ps.next()
                                    for q in range(4):
                                        h = hp * 4 + q
                                        K.emit('pe', lambda e, p2=p2, q=q, h=h, j=j: e.transpose(
                                            p2[:, q * 128:(q + 1) * 128], vT[:, h, j * 128:(j + 1) * 128], self.ident[:, :]),
                                            reads=[vT.k, self.ident.k], writes=[p2.k])
                                    if hp:
                                        K.emit('act', lambda e, p2=p2, vt=vt: e.activation(vt[:, 512:1024], p2[:, :], AF.Copy),
                                               reads=[p2.k], writes=[vt.k])
                                    else:
                                        K.emit('dve', lambda e, p2=p2, vt=vt: e.tensor_copy(vt[:, 0:512], p2[:, :]),
                                               reads=[p2.k], writes=[vt.k])
                                K.emit(Q2, lambda e, vt=vt, j=j, b0=b0: e.dma_start(
                                    out=S['V'][b0 + j * 128:b0 + (j + 1) * 128, :], in_=vt[:, :]),
                                    reads=[vt.k], writes=[self.dk['V'][bi][j]], lane=vt.name)
                    elif kind == 'r':
                        p = ps.next()
                        self.mm_group(p, p[:, :n], wt, hT, KC, n)
                        sb = st32.next()
                        K.emit('act', lambda e, p=p, sb=sb: e.activation(sb[:, :n], p[:, :n], AF.Silu),
                               reads=[p.k], writes=[sb.k])
                        K.emit(Q2, lambda e, sb=sb, idx=idx, b0=b0, n=n: e.dma_start(
                            out=S['RS'][:, idx, b0:b0 + n], in_=sb[:, :n]),
                            reads=[sb.k], writes=[self.dk['RS'][bi][idx]], lane=sb.name)
                    elif kind == 'u':
                        p = ps.next()
                        self.mm_group(p, p[:, :n], wt, hT, KC, n)
                        sb = st32.next()
                        self.gelu_evac(p[:, :n], p.k, sb[:, :n], sb.k, tmpa, tmpb, n)
                        K.emit(Q2, lambda e, sb=sb, idx=idx, b0=b0, n=n: e.dma_start(
                            out=S['UG'][:, idx, b0:b0 + n], in_=sb[:, :n]),
                            reads=[sb.k], writes=[self.dk['UG'][bi][idx]], lane=sb.name)
                    elif kind == 's':
                        p = ps.next()
                        self.mm_group(p, p[:, :n], wt, hT, KC, n)
                        self.gelu_evac(p[:, :n], p.k, gS[:, idx, :n], gS.k, tmpa, tmpb, n)
                        if idx == NH - 1:
                            for j in range(n // 128):
                                pa = ps.next()
                                pb = ps.next()
                                for h in range(NH):
                                    pp = pa if h < 4 else pb
                                    q = h % 4
                                    K.emit('pe', lambda e, pp=pp, q=q, h=h, j=j: e.transpose(
                                        pp[:, q * 128:(q + 1) * 128], gS[:, h, j * 128:(j + 1) * 128], self.ident[:, :]),
                                        reads=[gS.k, self.ident.k], writes=[pp.k])
                                lt = lnt.next()
                                K.emit('act', lambda e, pa=pa, lt=lt: e.activation(lt[:, 0:512], pa[:, :], AF.Copy),
                                       reads=[pa.k], writes=[lt.k])
                                K.emit('act', lambda e, pb=pb, lt=lt: e.activation(lt[:, 512:1024], pb[:, :], AF.Copy),
                                       reads=[pb.k], writes=[lt.k])
                                for q in range(2):
                                    K.emit('dve', lambda e, lt=lt, q=q: e.bn_stats(stat[:, q, :], lt[:, q * 512:(q + 1) * 512]),
                                           reads=[lt.k], writes=[stat.k])
                                K.emit('dve', lambda e: e.bn_aggr(mv[:, :], stat[:, :, :].rearrange("p a b -> p (a b)")), reads=[stat.k], writes=[mv.k])
                                K.emit('act', lambda e: e.activation(mv[:, 1:2], mv[:, 1:2], AF.Sqrt, bias=self.eps_t[:, 0:1]),
                                       reads=[mv.k, self.eps_t.k], writes=[mv.k])
                                K.emit('dve', lambda e: e.reciprocal(mv[:, 1:2], mv[:, 1:2]), reads=[mv.k], writes=[mv.k])
                                K.emit('dve', lambda e, lt=lt: e.tensor_scalar(
                                    out=lt[:, :], in0=lt[:, :], scalar1=mv[:, 0:1], scalar2=mv[:, 1:2],
                                    op0=ALU.subtract, op1=ALU.mult), reads=[lt.k, mv.k], writes=[lt.k])
                                K.emit('dve', lambda e, lt=lt: e.tensor_tensor(out=lt[:, :], in0=lt[:, :], in1=lng[:, :], op=ALU.mult),
                                       reads=[lt.k, lng.k], writes=[lt.k])
                                vs = vsn.next()
                                K.emit('dve', lambda e, lt=lt, vs=vs: e.tensor_tensor(out=vs[:, :], in0=lt[:, :], in1=lnb[:, :], op=ALU.add),
                                       reads=[lt.k, lnb.k], writes=[vs.k])
                                K.emit(Q2, lambda e, vs=vs, j=j, b0=b0: e.dma_start(
                                    out=S['VSN'][b0 + j * 128:b0 + (j + 1) * 128, :], in_=vs[:, :]),
                                    reads=[vs.k], writes=[self.dk['VSN'][bi][j]], lane=vs.name)

    def phaseB(self, l):
        K, I, S = self.K, self.I, self.Sx
        NT = self.NT
        nct = self.CTX // 128
        with self.phase():
            wup = Buf(K, 'bwup', [33, 2, 512], F32)
            K.emit(Q2, lambda e: e.dma_start(out=wup[:, :, :], in_=I['wup'][l].rearrange("d r c -> r d c")),
                   writes=[wup.k], lane=wup.name)
            identb = Buf(K, 'bidb', [64, 64], F32)
            qkb = Ring(K, 'bqk', 2, [64, 16, TB], F32)
            zlb = Ring(K, 'bzl', 2, [33, TB], F32)
            vb = Ring(K, 'bv', 2, [128, TB // 128, 1024], BF16)
            ob = Ring(K, 'bo', 2, [128, NH, TB], F32)
            S32 = Buf(K, 'bS32', [64, NH, 128], F32)
            Sbf = Buf(K, 'bSbf', [64, NH, 128], BF16)
            lz = Buf(K, 'blz', [64, NH, 128], F32)
            Gl = Buf(K, 'bGl', [64, NH, 128], F32)
            bcs = Buf(K, 'bbcs', [64, NH, 128], F32)
            eq = Buf(K, 'beq', [64, NH, 128], F32)
            ek = Buf(K, 'bek', [64, NH, 128], F32)
            qe = Buf(K, 'bqe', [64, NH, 128], BF16)
            ke32 = Buf(K, 'bke32', [64, NH, 128], F32)
            kebf = Buf(K, 'bkebf', [64, NH, 128], BF16)
            ketok = Buf(K, 'bketok', [128, NH, 64], BF16)
            atm = Buf(K, 'batm', [128, NH, 128], BF16)
            pzu = [Buf(K, f'bpzu{i}', [128, 512], F32, psum=True) for i in range(2)]
            pkt = Buf(K, 'bpkt', [128, 512], F32, psum=True)
            pat = [Buf(K, f'bpat{i}', [128, 512], F32, psum=True) for i in range(2)]
            po = [Buf(K, f'bpo{i}', [128, 512], F32, psum=True) for i in range(2)]

            def v4(b):
                return b.rearrange("p (h t) -> p h t", h=4)

            for d in range(2):
                mask = self.maskF if d == 0 else self.maskB
                if d == 0:
                    order = list(range(NT))
                else:
                    order = list(range(nct - 1, -1, -1)) + list(range(NT - 1, nct - 1, -1))
                K.emit('dve', lambda e: e.memset(S32[:, :, :], 0.0), writes=[S32.k])
                K.emit('dve', lambda e: e.memset(Sbf[:, :, :], 0.0), writes=[Sbf.k])
                curb = -1
                for ti in order:
                    bi = self.blk_of_tile(ti)
                    b0, n, s = self.blocks[bi]
                    if bi != curb:
                        if curb >= 0:
                            pb0, pn, _ = self.blocks[curb]
                            K.emit(Q2, lambda e, o=o, pb0=pb0, pn=pn: e.dma_start(out=S['OT'][:, :, pb0:pb0 + pn], in_=o[:, :, :pn]),
                                   reads=[o.k], writes=self.dk['OT'][curb], lane=o.name)
                        curb = bi
                        qk = qkb.next()
                        zl = zlb.next()
                        v = vb.next()
                        o = ob.next()
                        K.emit(Q2, lambda e, qk=qk, b0=b0, n=n: e.dma_start(out=qk[:, :, :n], in_=S['QK'][:, :, b0:b0 + n]),
                               reads=self.dk['QK'][bi], writes=[qk.k], lane=qk.name)
                        K.emit(Q2, lambda e, zl=zl, b0=b0, n=n: e.dma_start(out=zl[:, :n], in_=S['ZL'][:, b0:b0 + n]),
                               reads=self.dk['ZL'][bi], writes=[zl.k], lane=zl.name)
                        K.emit(Q2, lambda e, v=v, b0=b0, n=n: e.dma_start(
                            out=v[:, 0:n // 128, :], in_=S['V'][b0:b0 + n, :].rearrange("(j p) c -> p j c", p=128)),
                            reads=self.dk['V'][bi], writes=[v.k], lane=v.name)
                        if d == 1:
                            K.emit(Q2, lambda e, o=o, b0=b0, n=n: e.dma_start(out=o[:, :, :n], in_=S['OT'][:, :, b0:b0 + n]),
                                   reads=self.dk['OT'][bi], writes=[o.k], lane=o.name)
                    j = (ti * 128 - b0) // 128
                    c0, c1 = j * 128, (j + 1) * 128
                    for h in range(NH):
                        pz = pzu[h // 4]
                        K.emit('pe', lambda e, pz=pz, h=h, zl=zl, c0=c0, c1=c1: e.matmul(
                            pz[0:64, (h % 4) * 128:(h % 4 + 1) * 128], wup[:, d, h * 64:(h + 1) * 64], zl[:, c0:c1],
                            start=True, stop=True), reads=[wup.k, zl.k], writes=[pz.k])
                    for hp in range(2):
                        K.emit('act', lambda e, hp=hp: e.activation(lz[:, hp * 4:(hp + 1) * 4, :], v4(pzu[hp][0:64, :]), AF.Exp, scale=-1.0),
                               reads=[pzu[hp].k], writes=[lz.k])
                    K.emit('act', lambda e: e.activation(lz[:, :, :], lz[:, :, :], AF.Ln, bias=self.ones32[0:64, 0:1]),
                           reads=[lz.k, self.ones32.k], writes=[lz.k])
                    for h in range(NH):
                        K.emit('dve', lambda e, h=h: e.tensor_tensor_scan(
                            out=Gl[:, h, :], data0=self.ones32[0:64, :], data1=lz[:, h, :], initial=0.0,
                            op0=ALU.mult, op1=ALU.add), reads=[lz.k, self.ones32.k], writes=[Gl.k])
                    if d == 0:
                        src = Gl
                    else:
                        K.emit('dve', lambda e: e.tensor_tensor(out=bcs[:, :, :], in0=lz[:, :, :], in1=Gl[:, :, :], op=ALU.subtract),
                               reads=[lz.k, Gl.k], writes=[bcs.k])
                        K.emit('dve', lambda e: e.tensor_tensor(
                            out=bcs[:, :, :], in0=bcs[:, :, :], in1=Gl[:, :, 127:128].to_broadcast([64, NH, 128]), op=ALU.add),
                            reads=[bcs.k, Gl.k], writes=[bcs.k])
                        src = bcs
                    K.emit('act', lambda e, src=src: e.activation(eq[:, :, :], src[:, :, :], AF.Exp, scale=-1.0 / 16.0),
                           reads=[src.k], writes=[eq.k])
                    K.emit('act', lambda e, src=src: e.activation(ek[:, :, :], src[:, :, :], AF.Exp, scale=1.0 / 16.0),
                           reads=[src.k], writes=[ek.k])
                    for hp in range(2):
                        hs = slice(hp * 4, hp * 4 + 4)
                        K.emit('dve', lambda e, hs=hs, qk=qk, c0=c0, c1=c1: e.scalar_tensor_tensor(
                            out=qe[:, hs, :], in0=qk[:, hs, c0:c1], scalar=0.125, in1=eq[:, hs, :],
                            op0=ALU.mult, op1=ALU.mult), reads=[qk.k, eq.k], writes=[qe.k])
                    K.emit('dve', lambda e, qk=qk, c0=c0, c1=c1: e.tensor_tensor(
                        out=ke32[:, :, :], in0=qk[:, 8:16, c0:c1], in1=ek[:, :, :], op=ALU.mult),
                        reads=[qk.k, ek.k], writes=[ke32.k])
                    K.emit('act', lambda e: e.activation(kebf[:, :, :], ke32[:, :, :], AF.Copy), reads=[ke32.k], writes=[kebf.k])
                    for h in range(NH):
                        K.emit('pe', lambda e, h=h: e.transpose(pkt[:, h * 64:(h + 1) * 64], ke32[:, h, :], self.ident[0:64, 0:64]),
                               reads=[ke32.k, self.ident.k], writes=[pkt.k])
                    K.emit('act', lambda e: e.activation(ketok[:, :, :], pkt[:, :].rearrange("p (h k) -> p h k", h=NH), AF.Copy),
                           reads=[pkt.k], writes=[ketok.k])
                    for h in range(NH):
                        pa = pat[h // 4]
                        K.emit('pe', lambda e, pa=pa, h=h: e.matmul(
                            pa[:, (h % 4) * 128:(h % 4 + 1) * 128], kebf[:, h, :], qe[:, h, :], start=True, stop=True),
                            reads=[kebf.k, qe.k], writes=[pa.k])
                    for hp in range(2):
                        K.emit('dve', lambda e, hp=hp, mask=mask: e.tensor_tensor(
                            out=atm[:, hp * 4:(hp + 1) * 4, :], in0=v4(pat[hp][:, :]),
                            in1=mask[:, :].unsqueeze(1).to_broadcast([128, 4, 128]), op=ALU.mult),
                            reads=[pat[hp].k, mask.k], writes=[atm.k])
                    for h in range(NH):
                        pp = po[h // 4]
                        osl = pp[:, (h % 4) * 128:(h % 4 + 1) * 128]
                        K.emit('pe', lambda e, osl=osl, h=h, v=v, j=j: e.matmul(
                            osl, v[:, j, h * 128:(h + 1) * 128], atm[:, h, :], start=True, stop=False),
                            reads=[v.k, atm.k], writes=[pp.k])
                        K.emit('pe', lambda e, osl=osl, h=h: e.matmul(
                            osl, Sbf[:, h, :], qe[:, h, :], start=False, stop=True),
                            reads=[Sbf.k, qe.k], writes=[pp.k])
                    for hp in range(2):
                        hs = slice(hp * 4, hp * 4 + 4)
                        if d == 0:
                            K.emit('act', lambda e, hp=hp, hs=hs, o=o, c0=c0, c1=c1: e.activation(
                                o[:, hs, c0:c1], v4(po[hp][:, :]), AF.Copy), reads=[po[hp].k], writes=[o.k])
                        else:
                            K.emit('dve', lambda e, hp=hp, hs=hs, o=o, c0=c0, c1=c1: e.tensor_tensor(
                                out=o[:, hs, c0:c1], in0=o[:, hs, c0:c1], in1=v4(po[hp][:, :]), op=ALU.add),
                                reads=[po[hp].k, o.k], writes=[o.k])
                    for h in range(NH):
                        pz = pzu[h // 4]
                        K.emit('pe', lambda e, pz=pz, h=h, v=v, j=j: e.matmul(
                            pz[0:64, (h % 4) * 128:(h % 4 + 1) * 128], ketok[:, h, :], v[:, j, h * 128:(h + 1) * 128],
                            start=True, stop=True), reads=[ketok.k, v.k], writes=[pz.k])
                    ecol = 127 if d == 0 else 0
                    for hp in range(2):
                        hs = slice(hp * 4, hp * 4 + 4)
                        K.emit('dve', lambda e, hp=hp, hs=hs: e.tensor_tensor(
                            out=S32[:, hs, :], in0=S32[:, hs, :], in1=v4(pzu[hp][0:64, :]), op=ALU.add),
                            reads=[pzu[hp].k, S32.k], writes=[S32.k])
                    K.emit('dve', lambda e, ecol=ecol: e.tensor_tensor(
                        out=S32[:, :, :], in0=S32[:, :, :], in1=eq[:, :, ecol:ecol + 1].to_broadcast([64, NH, 128]), op=ALU.mult),
                        reads=[S32.k, eq.k], writes=[S32.k])
                    K.emit('act', lambda e: e.activation(Sbf[:, :, :], S32[:, :, :], AF.Copy), reads=[S32.k], writes=[Sbf.k])
                pb0, pn, _ = self.blocks[curb]
                K.emit(Q2, lambda e, o=o, pb0=pb0, pn=pn: e.dma_start(out=S['OT'][:, :, pb0:pb0 + pn], in_=o[:, :, :pn]),
                       reads=[o.k], writes=self.dk['OT'][curb], lane=o.name)

    def phaseC(self, l):
        K, I, S = self.K, self.I, self.Sx
        D, KC = self.D, self.KC
        last = (l == self.L - 1)
        with self.phase():
            hT = Buf(K, 'chT', [128, KC, TB], BF16)
            mT = Buf(K, 'cmT', [128, KC, TB], BF16)
            aT = Buf(K, 'caT', [128, NH, TB], BF16)
            sT = Buf(K, 'csT', [128, NH, TB], BF16)
            w8 = Ring(K, 'cw8', 6, [128, KC, 128], BF16)
            w2 = Ring(K, 'cw2', 6, [128, 8, 128], BF16)
            ld = Ring(K, 'cld', 4, [128, TB], F32)
            t32 = Ring(K, 'ct32', 3, [128, TB], F32)
            sqb = Buf(K, 'csq', [128, TB], BF16)
            rr = Buf(K, 'crr', [128, TB], F32)
            vsn = Buf(K, 'cvsn', [128, TB // 128, 1024], BF16)
            wsT = Buf(K, 'cwsT', [128, NH, 128], BF16)
            sgb = Buf(K, 'csgb', [128, NH, 128], F32)
            glag = Buf(K, 'cglag', [128, NH], F32)
            xr = Ring(K, 'cx', 3, [128, TB], F32)
            ps = self.psum_pool(8)
            K.emit(Q2, lambda e: e.dma_start(out=wsT[:, :, :], in_=S['wsT'][l]), reads=[self.wk[('wsT', l)]],
                   writes=[wsT.k], lane=wsT.name)
            K.emit(Q2, lambda e: e.dma_start(out=sgb[:, :, :].rearrange("p g i -> p (g i)"), in_=I['sgb'][l].partition_broadcast(128)),
                   writes=[sgb.k], lane=sgb.name)
            K.emit(Q2, lambda e: e.dma_start(out=glag[:, :], in_=I['glag'][l]), writes=[glag.k], lane=glag.name)
            for bi, (b0, n, s) in enumerate(self.blocks):
                if last and s == 0:
                    continue
                K.emit(Q2, lambda e, b0=b0, n=n: e.dma_start(out=hT[:, :, :n], in_=S['HT'][:, :, b0:b0 + n]),
                       reads=self.dk['HT'][bi], writes=[hT.k], lane=hT.name)
                K.emit(Q2, lambda e, b0=b0, n=n: e.dma_start(
                    out=vsn[:, 0:n // 128, :], in_=S['VSN'][b0:b0 + n, :].rearrange("(j p) c -> p j c", p=128)),
                    reads=self.dk['VSN'][bi], writes=[vsn.k], lane=vsn.name)
                for h in range(NH):
                    oh = ld.next()
                    K.emit(Q2, lambda e, oh=oh, h=h, b0=b0, n=n: e.dma_start(out=oh[:, :n], in_=S['OT'][:, h, b0:b0 + n]),
                           reads=self.dk['OT'][bi], writes=[oh.k], lane=oh.name)
                    rh = ld.next()
                    K.emit(Q2, lambda e, rh=rh, h=h, b0=b0, n=n: e.dma_start(out=rh[:, :n], in_=S['RS'][:, h, b0:b0 + n]),
                           reads=[self.dk['RS'][bi][h]], writes=[rh.k], lane=rh.name)
                    K.emit('act', lambda e, oh=oh: e.activation(sqb[:, :n], oh[:, :n], AF.Square), reads=[oh.k], writes=[sqb.k])
                    p = ps.next()
                    K.emit('pe', lambda e, p=p: e.matmul(p[:, :n], self.ones_bf[:, :], sqb[:, :n], start=True, stop=True),
                           reads=[sqb.k, self.ones_bf.k], writes=[p.k])
                    K.emit('act', lambda e, p=p: e.activation(rr[:, :n], p[:, :n], AF.Sqrt, bias=self.eps_t[:, 0:1], scale=1.0 / 128.0),
                           reads=[p.k, self.eps_t.k], writes=[rr.k])
                    K.emit('dve', lambda e: e.reciprocal(rr[:, :n], rr[:, :n]), reads=[rr.k], writes=[rr.k])
                    K.emit('dve', lambda e, oh=oh: e.tensor_tensor(out=oh[:, :n], in0=oh[:, :n], in1=rr[:, :n], op=ALU.mult),
                           reads=[oh.k, rr.k], writes=[oh.k])
                    K.emit('dve', lambda e, oh=oh, rh=rh, h=h: e.scalar_tensor_tensor(
                        out=aT[:, h, :n], in0=oh[:, :n], scalar=glag[:, h:h + 1], in1=rh[:, :n], op0=ALU.mult, op1=ALU.mult),
                        reads=[oh.k, rh.k, glag.k], writes=[aT.k])
                for g in range(NH):
                    ug = ld.next()
                    K.emit(Q2, lambda e, ug=ug, g=g, b0=b0, n=n: e.dma_start(out=ug[:, :n], in_=S['UG'][:, g, b0:b0 + n]),
                           reads=[self.dk['UG'][bi][g]], writes=[ug.k], lane=ug.name)
                    p = ps.next()
                    for j in range(n // 128):
                        K.emit('pe', lambda e, p=p, j=j, g=g: e.matmul(
                            p[:, j * 128:(j + 1) * 128], vsn[:, j, g * 128:(g + 1) * 128], wsT[:, g, :], start=True, stop=True),
                            reads=[vsn.k, wsT.k], writes=[p.k])
                    tt = t32.next()
                    nj = n // 128
                    K.emit('dve', lambda e, p=p, tt=tt, g=g, nj=nj: e.tensor_tensor(
                        out=tt[:, :n].rearrange("p (j i) -> p j i", j=nj), in0=p[:, :n].rearrange("p (j i) -> p j i", j=nj),
                        in1=sgb[:, g, :].unsqueeze(1).to_broadcast([128, nj, 128]), op=ALU.add),
                        reads=[p.k, sgb.k], writes=[tt.k])
                    K.emit('dve', lambda e, tt=tt, ug=ug, g=g: e.tensor_tensor(out=sT[:, g, :n], in0=tt[:, :n], in1=ug[:, :n], op=ALU.mult),
                           reads=[tt.k, ug.k], writes=[sT.k])
                for ncx in range(KC):
                    wga = w8.next()
                    K.emit('sp', lambda e, wga=wga, ncx=ncx: e.dma_start(out=wga[:, :, :], in_=S['win'][l][41 + ncx]),
                           reads=[self.wk[('winG', l)]], writes=[wga.k], lane=wga.name)
                    wgb = w8.next()
                    K.emit('sp', lambda e, wgb=wgb, ncx=ncx: e.dma_start(out=wgb[:, :, :], in_=S['win'][l][41 + KC + ncx]),
                           reads=[self.wk[('winG', l)]], writes=[wgb.k], lane=wgb.name)
                    wa = w2.next()
                    K.emit('sp', lambda e, wa=wa, ncx=ncx: e.dma_start(out=wa[:, :, :], in_=S['wa'][l][ncx]),
                           reads=[self.wk[('wa', l)]], writes=[wa.k], lane=wa.name)
                    wb = w2.next()
                    K.emit('sp', lambda e, wb=wb, ncx=ncx: e.dma_start(out=wb[:, :, :], in_=S['wb'][l][ncx]),
                           reads=[self.wk[('wb', l)]], writes=[wb.k], lane=wb.name)
                    pga, pgb, pya, pyb = ps.next(), ps.next(), ps.next(), ps.next()
                    self.mm_group(pga, pga[:, :n], wga, hT, KC, n)
                    self.mm_group(pya, pya[:, :n], wa, aT, 8, n)
                    self.mm_group(pgb, pgb[:, :n], wgb, hT, KC, n)
                    self.mm_group(pyb, pyb[:, :n], wb, sT, 8, n)
                    ta = t32.next()
                    tb = t32.next()
                    K.emit('act', lambda e, pga=pga, ta=ta: e.activation(ta[:, :n], pga[:, :n], AF.Sigmoid), reads=[pga.k], writes=[ta.k])
                    K.emit('act', lambda e, pgb=pgb, tb=tb: e.activation(tb[:, :n], pgb[:, :n], AF.Sigmoid), reads=[pgb.k], writes=[tb.k])
                    K.emit('dve', lambda e, ta=ta, pya=pya: e.tensor_tensor(out=ta[:, :n], in0=ta[:, :n], in1=pya[:, :n], op=ALU.mult),
                           reads=[ta.k, pya.k], writes=[ta.k])
                    K.emit('dve', lambda e, tb=tb, pyb=pyb: e.tensor_tensor(out=tb[:, :n], in0=tb[:, :n], in1=pyb[:, :n], op=ALU.mult),
                           reads=[tb.k, pyb.k], writes=[tb.k])
                    K.emit('dve', lambda e, ta=ta, tb=tb, ncx=ncx: e.tensor_tensor(out=mT[:, ncx, :n], in0=ta[:, :n], in1=tb[:, :n], op=ALU.add),
                           reads=[ta.k, tb.k], writes=[mT.k])
                for ec in range(KC):
                    wo = w8.next()
                    K.emit('sp', lambda e, wo=wo, ec=ec: e.dma_start(out=wo[:, :, :], in_=S['wo'][l][ec]),
                           reads=[self.wk[('wo', l)]], writes=[wo.k], lane=wo.name)
                    xb = xr.next()
                    K.emit(Q2, lambda e, xb=xb, ec=ec, b0=b0, n=n: e.dma_start(out=xb[:, :n], in_=self.xt1(ec, b0, b0 + n)),
                           reads=[self.dk['XT'][bi][ec]], writes=[xb.k], lane=xb.name)
                    p = ps.next()
                    self.mm_group(p, p[:, :n], wo, mT, KC, n)
                    K.emit('dve', lambda e, p=p, xb=xb, ec=ec, s=s: e.scalar_tensor_tensor(
                        out=xb[:, :n], in0=p[:, :n], scalar=self.mod[:, 2, ec, s:s + 1], in1=xb[:, :n], op0=ALU.mult, op1=ALU.add),
                        reads=[p.k, xb.k, self.mod.k], writes=[xb.k])
                    K.emit(Q2, lambda e, xb=xb, ec=ec, b0=b0, n=n: e.dma_start(out=self.xt1(ec, b0, b0 + n), in_=xb[:, :n]),
                           reads=[xb.k], writes=[self.dk['XT'][bi][ec]], lane=xb.name)

    def phaseD(self, l):
        K, I, S = self.K, self.I, self.Sx
        D, KC = self.D, self.KC
        last = (l == self.L - 1)
        with self.phase():
            xr = Ring(K, 'dx', 2, [128, min(4, KC), TB], F32)
            sqr = Ring(K, 'dsq', 2, [128, TB], BF16)
            rs = Buf(K, 'drs', [128, TB], F32)
            hT = Ring(K, 'dhT', 2, [128, KC, TB], BF16)
            wrt = Buf(K, 'dwr', [128, KC, NE], BF16)
            ex = Ring(K, 'dex', 2, [NE, TB], F32)
            rsum = Buf(K, 'drsum', [NE, TB], F32)
            ps = self.psum_pool(6)
            K.emit(Q2, lambda e: e.dma_start(out=wrt[:, :, :], in_=S['wr'][l]), reads=[self.wk[('wr', l)]],
                   writes=[wrt.k], lane=wrt.name)
            for bi, (b0, n, s) in enumerate(self.blocks):
                if last and s == 0:
                    continue
                psn = ps.next()
                h = hT.next()
                self.norm_stats(S['XT'], bi, xr, sqr, psn, rs, float(D))
                self.norm_apply(S['XT'], bi, xr, rs, self.A2, _ModView(self.mod, 3), h, s)
                K.emit(Q2, lambda e, h=h, b0=b0, n=n: e.dma_start(out=S['HT'][:, :, b0:b0 + n], in_=h[:, :, :n]),
                       reads=[h.k], writes=self.dk['HT'][bi], lane=h.name)
                p = ps.next()
                self.mm_group(p, p[0:NE, :n], wrt, h, KC, n, M=NE, w0=0)
                x = ex.next()
                K.emit('act', lambda e, p=p, x=x: e.activation(x[:, :n], p[0:NE, :n], AF.Exp), reads=[p.k], writes=[x.k])
                p2 = ps.next()
                K.emit('pe', lambda e, p2=p2, x=x: e.matmul(p2[0:NE, :n], self.ones32[0:NE, 0:NE], x[:, :n], start=True, stop=True),
                       reads=[x.k, self.ones32.k], writes=[p2.k])
                K.emit('dve', lambda e, p2=p2: e.reciprocal(rsum[:, :n], p2[0:NE, :n]), reads=[p2.k], writes=[rsum.k])
                K.emit('dve', lambda e, x=x: e.tensor_tensor(out=x[:, :n], in0=x[:, :n], in1=rsum[:, :n], op=ALU.mult),
                       reads=[x.k, rsum.k], writes=[x.k])
                affd = S['AFFC'][:, 0:n] if s == 0 else S['AFFL'][:, b0 - self.CTX:b0 - self.CTX + n]
                K.emit(Q2, lambda e, x=x, affd=affd, n=n: e.dma_start(out=affd, in_=x[:, :n]),
                       reads=[x.k], writes=self.dk['AFF'][bi], lane=x.name)

    def phaseE(self, l):
        K, I, S = self.K, self.I, self.Sx
        SEQ, CTX = self.SEQ, self.CTX
        last = (l == self.L - 1)
        nl, ncx = SEQ // 8, CTX // 8
        with self.phase():
            aff = Buf(K, 'eaff', [128, nl + ncx], F32)
            junk = Buf(K, 'ejunk', [128, nl], F32)
            bsel = Buf(K, 'ebsel', [128, 128], F32)
            lo = Buf(K, 'elo', [128, 2], F32)
            hi = Buf(K, 'ehi', [128, 2], F32)
            mid = Buf(K, 'emid', [128, 2], F32)
            cnt = Buf(K, 'ecnt', [128, 2], F32)
            ge = Buf(K, 'ege', [128, 2], F32)
            dd = Buf(K, 'edd', [128, 2], F32)
            kv = Buf(K, 'ekv', [128, 2], F32)
            pc = Buf(K, 'epc', [128, 512], F32, psum=True)
            K.emit(Q2, lambda e: e.dma_start(out=bsel[:, :], in_=I['bsel']), writes=[bsel.k], lane=bsel.name)
            K.emit(Q2, lambda e: e.dma_start(out=aff[:, 0:nl], in_=S['AFFL'].rearrange("e (s i) -> (e s) i", s=8)),
                   reads=[t for b in self.dk['AFF'][1:] for t in b], writes=[aff.k], lane=aff.name)
            if not last:
                K.emit(Q2, lambda e: e.dma_start(out=aff[:, nl:nl + ncx], in_=S['AFFC'].rearrange("e (s i) -> (e s) i", s=8)),
                       reads=self.dk['AFF'][0], writes=[aff.k], lane=aff.name)
            else:
                K.emit('dve', lambda e: e.memset(aff[:, nl:nl + ncx], 0.0), writes=[aff.k])
            K.emit('dve', lambda e: e.memset(lo[:, :], 0.0), writes=[lo.k])
            K.emit('dve', lambda e: e.memset(hi[:, :], 2.0), writes=[hi.k])
            K.emit('dve', lambda e: e.memset(kv[:, 0:1], float(SEQ // 8)), writes=[kv.k])
            K.emit('dve', lambda e: e.memset(kv[:, 1:2], float(CTX // 8)), writes=[kv.k])
            for it in range(NBIS):
                K.emit('dve', lambda e: e.tensor_tensor(out=mid[:, :], in0=lo[:, :], in1=hi[:, :], op=ALU.add),
                       reads=[lo.k, hi.k], writes=[mid.k])
                K.emit('dve', lambda e: e.tensor_scalar(out=mid[:, :], in0=mid[:, :], scalar1=0.5, scalar2=None, op0=ALU.mult),
                       reads=[mid.k], writes=[mid.k])
                K.emit('dve', lambda e: e.tensor_scalar(out=junk[:, 0:nl], in0=aff[:, 0:nl], scalar1=mid[:, 0:1], scalar2=0.0,
                                                        op0=ALU.is_ge, op1=ALU.add, accum_out=cnt[:, 0:1]),
                       reads=[aff.k, mid.k], writes=[junk.k, cnt.k])
                K.emit('dve', lambda e: e.tensor_scalar(out=junk[:, 0:ncx], in0=aff[:, nl:nl + ncx], scalar1=mid[:, 1:2], scalar2=0.0,
                                                        op0=ALU.is_ge, op1=ALU.add, accum_out=cnt[:, 1:2]),
                       reads=[aff.k, mid.k], writes=[junk.k, cnt.k])
                K.emit('pe', lambda e: e.matmul(pc[:, 0:2], bsel[:, :], cnt[:, :], start=True, stop=True),
                       reads=[bsel.k, cnt.k], writes=[pc.k])
                K.emit('dve', lambda e: e.tensor_tensor(out=ge[:, :], in0=pc[:, 0:2], in1=kv[:, :], op=ALU.is_ge),
                       reads=[pc.k, kv.k], writes=[ge.k])
                K.emit('dve', lambda e: e.tensor_tensor(out=dd[:, :], in0=mid[:, :], in1=lo[:, :], op=ALU.subtract),
                       reads=[mid.k, lo.k], writes=[dd.k])
                K.emit('dve', lambda e: e.tensor_tensor(out=dd[:, :], in0=dd[:, :], in1=ge[:, :], op=ALU.mult),
                       reads=[dd.k, ge.k], writes=[dd.k])
                K.emit('dve', lambda e: e.tensor_tensor(out=lo[:, :], in0=lo[:, :], in1=dd[:, :], op=ALU.add),
                       reads=[lo.k, dd.k], writes=[lo.k])
                K.emit('dve', lambda e: e.tensor_tensor(out=dd[:, :], in0=hi[:, :], in1=mid[:, :], op=ALU.subtract),
                       reads=[mid.k, hi.k], writes=[dd.k])
                K.emit('dve', lambda e: e.tensor_tensor(out=dd[:, :], in0=dd[:, :], in1=ge[:, :], op=ALU.mult),
                       reads=[dd.k, ge.k], writes=[dd.k])
                K.emit('dve', lambda e: e.tensor_tensor(out=hi[:, :], in0=mid[:, :], in1=dd[:, :], op=ALU.add),
                       reads=[mid.k, dd.k], writes=[hi.k])
            K.emit(Q2, lambda e: e.dma_start(out=S['THR'], in_=lo[:, :]), reads=[lo.k], writes=self.dk['THR'][0], lane=lo.name)

    def phaseF(self, l):
        K, I, S = self.K, self.I, self.Sx
        D, KC = self.D, self.KC
        last = (l == self.L - 1)
        with self.phase():
            hT = Buf(K, 'fhT', [128, KC, TB], BF16)
            hid = Buf(K, 'fhid', [128, 2 * NE, TB], BF16)
            w8 = Ring(K, 'fw8', 8, [128, KC, 128], BF16)
            w32 = Ring(K, 'fw32', 3, [128, 32, 128], BF16)
            sel = Buf(K, 'fsel', [NE, NE, 128], F32)
            thr = Buf(K, 'fthr', [NE, 2], F32)
            affb = Buf(K, 'faff', [NE, TB], F32)
            gm = Buf(K, 'fgm', [NE, TB], F32)
            t32 = Ring(K, 'ft32', 3, [128, TB], F32)
            xr = Ring(K, 'fx', 3, [128, TB], F32)
            ps = self.psum_pool(8)
            K.emit(Q2, lambda e: e.dma_start(out=sel[:, :, :], in_=I['sel']), writes=[sel.k], lane=sel.name)
            K.emit(Q2, lambda e: e.dma_start(out=thr[:, :], in_=S['THR'].rearrange("(e s) c -> e s c", s=8)[:, 0, :]),
                   reads=self.dk['THR'][0], writes=[thr.k], lane=thr.name)
            for bi, (b0, n, s) in enumerate(self.blocks):
                if last and s == 0:
                    continue
                K.emit(Q2, lambda e, b0=b0, n=n: e.dma_start(out=hT[:, :, :n], in_=S['HT'][:, :, b0:b0 + n]),
                       reads=self.dk['HT'][bi], writes=[hT.k], lane=hT.name)
                affs = S['AFFC'][:, 0:n] if s == 0 else S['AFFL'][:, b0 - self.CTX:b0 - self.CTX + n]
                K.emit(Q2, lambda e, affs=affs, n=n: e.dma_start(out=affb[:, :n], in_=affs),
                       reads=self.dk['AFF'][bi], writes=[affb.k], lane=affb.name)
                tc = 1 - s
                K.emit('dve', lambda e, tc=tc: e.scalar_tensor_tensor(
                    out=gm[:, :n], in0=affb[:, :n], scalar=thr[:, tc:tc + 1], in1=affb[:, :n], op0=ALU.is_ge, op1=ALU.mult),
                    reads=[affb.k, thr.k], writes=[gm.k])
                for ex in range(NE):
                    pg = ps.next()
                    K.emit('pe', lambda e, pg=pg, ex=ex: e.matmul(pg[:, :n], sel[:, ex, :], gm[:, :n], start=True, stop=True),
                           reads=[sel.k, gm.k], writes=[pg.k])
                    for fc in range(2):
                        w1 = w8.next()
                        K.emit('sp', lambda e, w1=w1, ex=ex, fc=fc: e.dma_start(out=w1[:, :, :], in_=S['w1'][l][2 * ex + fc]),
                               reads=[self.wk[('w1', l)]], writes=[w1.k], lane=w1.name)
                        w3 = w8.next()
                        K.emit('sp', lambda e, w3=w3, ex=ex, fc=fc: e.dma_start(out=w3[:, :, :], in_=S['w3'][l][2 * ex + fc]),
                               reads=[self.wk[('w3', l)]], writes=[w3.k], lane=w3.name)
                        p1, p3 = ps.next(), ps.next()
                        self.mm_group(p1, p1[:, :n], w1, hT, KC, n)
                        self.mm_group(p3, p3[:, :n], w3, hT, KC, n)
                        tt = t32.next()
                        K.emit('act', lambda e, p1=p1, tt=tt: e.activation(tt[:, :n], p1[:, :n], AF.Silu), reads=[p1.k], writes=[tt.k])
                        K.emit('dve', lambda e, p3=p3, tt=tt: e.tensor_tensor(out=tt[:, :n], in0=tt[:, :n], in1=p3[:, :n], op=ALU.mult),
                               reads=[tt.k, p3.k], writes=[tt.k])
                        K.emit('dve', lambda e, pg=pg, tt=tt, ex=ex, fc=fc: e.tensor_tensor(
                            out=hid[:, 2 * ex + fc, :n], in0=tt[:, :n], in1=pg[:, :n], op=ALU.mult),
                            reads=[tt.k, pg.k], writes=[hid.k])
                for dc in range(KC):
                    w2 = w32.next()
                    K.emit('sp', lambda e, w2=w2, dc=dc: e.dma_start(out=w2[:, :, :], in_=S['w2'][l][dc]),
                           reads=[self.wk[('w2', l)]], writes=[w2.k], lane=w2.name)
                    xb = xr.next()
                    K.emit(Q2, lambda e, xb=xb, dc=dc, b0=b0, n=n: e.dma_start(out=xb[:, :n], in_=self.xt1(dc, b0, b0 + n)),
                           reads=[self.dk['XT'][bi][dc]], writes=[xb.k], lane=xb.name)
                    p = ps.next()
                    self.mm_group(p, p[:, :n], w2, hid, 2 * NE, n)
                    K.emit('dve', lambda e, p=p, xb=xb, dc=dc, s=s: e.scalar_tensor_tensor(
                        out=xb[:, :n], in0=p[:, :n], scalar=self.mod[:, 5, dc, s:s + 1], in1=xb[:, :n], op0=ALU.mult, op1=ALU.add),
                        reads=[p.k, xb.k, self.mod.k], writes=[xb.k])
                    K.emit(Q2, lambda e, xb=xb, dc=dc, b0=b0, n=n: e.dma_start(out=self.xt1(dc, b0, b0 + n), in_=xb[:, :n]),
                           reads=[xb.k], writes=[self.dk['XT'][bi][dc]], lane=xb.name)

    def final(self):
        K, I, S = self.K, self.I, self.Sx
        D, KC, CTX = self.D, self.KC, self.CTX
        self.out_tok = Tok()
        with self.phase():
            xr = Ring(K, 'zx', 2, [128, min(4, KC), TB], F32)
            sqr = Ring(K, 'zsq', 2, [128, TB], BF16)
            rs = Buf(K, 'zrs', [128, TB], F32)
            fg = Buf(K, 'zfg', [128, KC], F32)
            yT = Buf(K, 'zyT', [128, KC, TB], F32)
            ost = Ring(K, 'zost', 2, [128, D], F32)
            ps = self.psum_pool(6)
            K.emit(Q2, lambda e: e.dma_start(out=fg[:, :], in_=I['fg']), writes=[fg.k], lane=fg.name)
            for bi, (b0, n, s) in enumerate(self.blocks):
                if s == 0:
                    continue
                psn = ps.next()
                self.norm_stats(S['XT'], bi, xr, sqr, psn, rs, float(D))
                self.norm_apply(S['XT'], bi, xr, rs, fg, None, yT, s)
                for j in range(n // 128):
                    o = ost.next()
                    for c0 in range(0, KC, 4):
                        p = ps.next()
                        for q in range(4):
                            kc = c0 + q
                            K.emit('pe', lambda e, p=p, q=q, kc=kc, j=j: e.transpose(
                                p[:, q * 128:(q + 1) * 128], yT[:, kc, j * 128:(j + 1) * 128], self.ident[:, :]),
                                reads=[yT.k, self.ident.k], writes=[p.k])
                        if (c0 // 4) % 2:
                            K.emit('act', lambda e, p=p, o=o, c0=c0: e.activation(o[:, c0 * 128:(c0 + 4) * 128], p[:, :], AF.Copy),
                                   reads=[p.k], writes=[o.k])
                        else:
                            K.emit('dve', lambda e, p=p, o=o, c0=c0: e.tensor_copy(o[:, c0 * 128:(c0 + 4) * 128], p[:, :]),
                                   reads=[p.k], writes=[o.k])
                    r0 = b0 - CTX + j * 128
                    K.emit(Q2, lambda e, o=o, r0=r0: e.dma_start(out=self.out[r0:r0 + 128, :], in_=o[:, :]),
                           reads=[o.k], writes=[self.out_tok], lane=o.name)


class _ModView:
    def __init__(self, mod, j):
        self.mod = mod
        self.j = j
        self.k = mod.k

    def __getitem__(self, idx):
        return self.mod.t[:, self.j][idx]


def host_inputs(D, SEQ, CTX, L, x, c, ctx, c_ctx, ada_w, ada_b, norm1_g, norm2_g, w_in, w_dec_up, b_dec,
                gla_norm_g, sg_ln_g, sg_ln_b, sg_w, sg_b, w_branch_a, w_branch_b, w_out,
                w_router, w_exp1, w_exp3, w_exp2, final_g):
    KC = D // 128
    f = lambda a: np.ascontiguousarray(np.asarray(a, dtype=np.float32))
    m = {}
    m['x'] = f(x)[0]
    m['ctx'] = f(ctx)[0]
    cc = np.stack([f(c_ctx), f(c)[0]], axis=-1)
    m['ccT'] = np.ascontiguousarray(cc.reshape(KC, 128, 2).transpose(1, 0, 2))
    m['ada_w'] = f(ada_w)
    m['ada_b'] = np.ascontiguousarray(f(ada_b).reshape(L, 6 * KC, 128).transpose(0, 2, 1))
    m['n1g'] = np.ascontiguousarray(f(norm1_g).reshape(L, KC, 128).transpose(0, 2, 1))
    m['n2g'] = np.ascontiguousarray(f(norm2_g).reshape(L, KC, 128).transpose(0, 2, 1))
    m['fg'] = np.ascontiguousarray(f(final_g).reshape(KC, 128).T)
    m['w_in'] = f(w_in)
    wup = np.zeros((L, 2, 33, 512), np.float32)
    wup[:, 0, 0:16] = f(w_dec_up)[:, 0]
    wup[:, 1, 16:32] = f(w_dec_up)[:, 1]
    wup[:, :, 32] = f(b_dec)
    m['wup'] = wup
    m['glag'] = np.ascontiguousarray(f(gla_norm_g).reshape(L, NH, 128).transpose(0, 2, 1))
    m['lng'] = f(sg_ln_g)
    m['lnb'] = f(sg_ln_b)
    m['wsT'] = np.ascontiguousarray(f(sg_w).transpose(0, 3, 1, 2))
    m['sgb'] = np.ascontiguousarray(f(sg_b).reshape(L, 1024))
    m['wa'] = f(w_branch_a)
    m['wb'] = f(w_branch_b)
    m['wo'] = f(w_out)
    m['wr'] = f(w_router)
    m['w1'] = f(w_exp1)
    m['w3'] = f(w_exp3)
    m['w2'] = np.ascontiguousarray(f(w_exp2).reshape(L, NE * FF, D))
    m['ident'] = np.eye(128, dtype=np.float32)
    jj, ii = np.meshgrid(np.arange(128), np.arange(128), indexing='ij')
    m['maskF'] = (jj <= ii).astype(np.float32)
    m['maskB'] = (jj >= ii).astype(np.float32)
    m['bsel'] = ((jj // 8) == (ii // 8)).astype(np.float32)
    sel = np.zeros((NE, NE, 128), np.float32)
    for e in range(NE):
        sel[e, e, :] = 1.0
    m['sel'] = sel
    return m


_CACHE = {}


def run(D, SEQ, CTX, L, inputs, debug=False):
    key = (D, SEQ, CTX, L, debug)
    if key not in _CACHE:
        _CACHE[key] = Builder(D, SEQ, CTX, L, debug).build()
    nc = _CACHE[key]
    m = host_inputs(D, SEQ, CTX, L, **inputs)
    res = run_bass_kernel_spmd(nc, [m], core_ids=[0])
    if debug:
        return res.results[0]
    return res.results[0]['out'].reshape(1, SEQ, D).astype(np.float32)


def kernel(**inputs):
    return run(4096, 16384, 256, 4, inputs)
```
